# Optimizing a Trainium2 kernel written in Bass

```python
import math
import jax, jax.numpy as jnp
from jax import lax
import numpy as np


D_MODEL = 2048
BATCH = 8
SEQ = 2048
DEPTH = 1

MEM_TOKENS = 256
RET_HEADS = 8
RET_QK_DIM = 128
RET_V_DIM = 128
RET_CHUNK = 128
DIL_PATTERNS = ((128, 1), (512, 4), (2048, 16))
DIL_HEADS_PER_GROUP = 4
DIL_HEAD_DIM = 128
DIL_BLOCK = 128
ROPE_THETA = 10000.0
MEM_HEADS = 4
MEM_HEAD_DIM = D_MODEL // MEM_HEADS
PEER_HEADS = 8
PEER_N_KEYS = 128
PEER_N_EXPERTS = PEER_N_KEYS * PEER_N_KEYS
PEER_QUERY_DIM = 256
PEER_TOPK = 16
PEER_TOKEN_BLOCK = 128
NORM_EPS = 1e-6

RET_QK_W = RET_HEADS * RET_QK_DIM
RET_V_W = RET_HEADS * RET_V_DIM
DIL_N_GROUPS = len(DIL_PATTERNS)
DIL_QKV_W = DIL_N_GROUPS * DIL_HEADS_PER_GROUP * DIL_HEAD_DIM
DIL_OUT_W = DIL_HEADS_PER_GROUP * DIL_HEAD_DIM
IN_SPLIT_SIZES = (RET_QK_W, RET_QK_W, RET_V_W, RET_V_W, DIL_QKV_W, DIL_QKV_W, DIL_QKV_W, D_MODEL, D_MODEL)
IN_WIDTH = sum(IN_SPLIT_SIZES)
IN_SPLIT_POINTS = tuple(int(c) for c in np.cumsum(IN_SPLIT_SIZES)[:-1])

kernel_name = 'hybrid_retention_dilated_peer_block'


def rms_norm(x, g):
    xf = x.astype(jnp.float32)
    y = xf * lax.rsqrt(jnp.mean(xf * xf, axis=-1, keepdims=True) + NORM_EPS)
    return (y * g.astype(jnp.float32)).astype(x.dtype)


def rotary(x, pos):
    dh = x.shape[-1]
    half = dh // 2
    inv = 1.0 / (ROPE_THETA ** (jnp.arange(half, dtype=jnp.float32) / half))
    ang = pos.astype(jnp.float32)[..., None] * inv
    cos = jnp.cos(ang)[:, :, None, :]
    sin = jnp.sin(ang)[:, :, None, :]
    xf = x.astype(jnp.float32)
    x1, x2 = xf[..., :half], xf[..., half:]
    return jnp.concatenate([x1 * cos - x2 * sin, x2 * cos + x1 * sin], axis=-1).astype(x.dtype)


def retnet_rotate(x, pos):
    dh = x.shape[-1]
    angle = 1.0 / (10000.0 ** jnp.linspace(0.0, 1.0, dh // 2, dtype=jnp.float32))
    angle = jnp.repeat(angle, 2)
    ang = pos.astype(jnp.float32)[..., None] * angle
    cos = jnp.cos(ang)[:, :, None, :]
    sin = jnp.sin(ang)[:, :, None, :]
    xf = x.astype(jnp.float32)
    rot = jnp.stack([-xf[..., 1::2], xf[..., 0::2]], axis=-1).reshape(xf.shape)
    return xf * cos + rot * sin


def retention(q, k, v, pos):
    b, s, h, dk = q.shape
    dv = v.shape[-1]
    c = RET_CHUNK
    n = s // c
    q = retnet_rotate(q, pos)
    k = retnet_rotate(k, pos) * (dk ** -0.5)
    v = v.astype(jnp.float32)
    to_chunks = lambda t: t.reshape(b, n, c, h, t.shape[-1]).transpose(0, 3, 1, 2, 4)
    qc, kc, vc = to_chunks(q), to_chunks(k), to_chunks(v)
    log_g = jnp.log1p(-jnp.exp2(-5.0 - jnp.arange(h, dtype=jnp.float32)))
    i = jnp.arange(c, dtype=jnp.float32)
    diff = i[:, None] - i[None, :]
    dmat = jnp.where(diff >= 0, jnp.exp(log_g[:, None, None] * jnp.maximum(diff, 0.0)), 0.0)
    scores = jnp.einsum('bhncd,bhnjd->bhncj', qc, kc) * dmat[None, :, None]
    inner = jnp.einsum('bhncj,bhnje->bhnce', scores, vc)
    k_w = jnp.exp(log_g[:, None] * (c - 1 - i))
    kv = jnp.einsum('bhncd,bhnce->bhnde', kc * k_w[None, :, None, :, None], vc)
    chunk_decay = jnp.exp(log_g * c)[None, :, None, None]

    def step(state, kv_n):
        return chunk_decay * state + kv_n, state

    init = jnp.zeros((b, h, dk, dv), kv.dtype)
    _, prev = lax.scan(step, init, jnp.moveaxis(kv, 2, 0))
    prev = jnp.moveaxis(prev, 0, 2)
    q_w = jnp.exp(log_g[:, None] * (i + 1.0))
    cross = jnp.einsum('bhncd,bhnde->bhnce', qc * q_w[None, :, None, :, None], prev)
    out = (inner + cross).transpose(0, 2, 3, 1, 4).reshape(b, s, h, dv)
    out = out * lax.rsqrt(jnp.mean(out * out, axis=-1, keepdims=True) + NORM_EPS)
    return out


def banded_attention(q, k, v, window):
    *lead, L, dh = q.shape
    blk = DIL_BLOCK
    nb = -(-L // blk)
    pad = nb * blk - L
    padw = [(0, 0)] * len(lead) + [(0, pad), (0, 0)]
    qb = jnp.pad(q, padw).reshape(*lead, nb, blk, dh)
    kb = jnp.pad(k, padw).reshape(*lead, nb, blk, dh)
    vb = jnp.pad(v, padw).reshape(*lead, nb, blk, dh)
    shift = lambda t: jnp.concatenate([jnp.zeros_like(t[..., :1, :, :]), t[..., :-1, :, :]], axis=-3)
    kcat = jnp.concatenate([shift(kb), kb], axis=-2)
    vcat = jnp.concatenate([shift(vb), vb], axis=-2)
    s = jnp.einsum('...nqd,...nkd->...nqk', qb, kcat).astype(jnp.float32) * (dh ** -0.5)
    nidx = jnp.arange(nb)[:, None, None]
    qpos = nidx * blk + jnp.arange(blk)[None, :, None]
    kpos = (nidx - 1) * blk + jnp.arange(2 * blk)[None, None, :]
    dist = qpos - kpos
    mask = (dist >= 0) & (dist <= window) & (kpos >= 0)
    s = jnp.where(mask, s, -jnp.inf)
    lse = jax.nn.logsumexp(s, axis=-1, keepdims=True)
    p = jnp.exp(s - lse)
    out = jnp.einsum('...nqk,...nkd->...nqd', p.astype(vcat.dtype), vcat)
    out = out.reshape(*lead, nb * blk, dh)[..., :L, :]
    lse = lse[..., 0].reshape(*lead, nb * blk)[..., :L]
    return out, lse


def dilated_attention(q, k, v):
    b, s, _, dh = q.shape
    hg = DIL_HEADS_PER_GROUP
    outs, lses = [], []
    for g, (w, r) in enumerate(DIL_PATTERNS):
        sl = slice(g * hg, (g + 1) * hg)
        sub = lambda t: t[:, :, sl].reshape(b, s // r, r, hg, dh).transpose(0, 2, 3, 1, 4)
        o, l = banded_attention(sub(q), sub(k), sub(v), w // r)
        outs.append(o.transpose(0, 3, 1, 2, 4).reshape(b, s, hg, dh))
        lses.append(l.transpose(0, 3, 1, 2).reshape(b, s, hg))
    outs = jnp.stack(outs, axis=0)
    wts = jax.nn.softmax(jnp.stack(lses, axis=0), axis=0)
    return jnp.sum(wts[..., None] * outs.astype(jnp.float32), axis=0)


def memory_cross_attention(h, memn, w_q, w_kv, w_o):
    b, s, d = h.shape
    m = memn.shape[1]
    q = (h @ w_q).reshape(b, s, MEM_HEADS, MEM_HEAD_DIM)
    k, v = jnp.split(memn @ w_kv, 2, axis=-1)
    k = k.reshape(b, m, MEM_HEADS, MEM_HEAD_DIM)
    v = v.reshape(b, m, MEM_HEADS, MEM_HEAD_DIM)
    sc = jnp.einsum('bshd,bmhd->bhsm', q, k).astype(jnp.float32) * (MEM_HEAD_DIM ** -0.5)
    p = jax.nn.softmax(sc, axis=-1)
    o = jnp.einsum('bhsm,bmhd->bshd', p.astype(v.dtype), v).reshape(b, s, d)
    return o @ w_o


def peer_ffn(h, w_q, subkeys, u_tab, v_tab):
    b, s, d = h.shape
    t = b * s
    ht = h.reshape(t, d)
    qh = (ht @ w_q).reshape(t, PEER_HEADS, 2, PEER_QUERY_DIM // 2)
    sc = jnp.einsum('thpd,hpnd->thpn', qh, subkeys).astype(jnp.float32)
    v_top, i_top = lax.top_k(sc, PEER_TOPK)
    combo = (v_top[:, :, 0, :, None] + v_top[:, :, 1, None, :]).reshape(t, PEER_HEADS, PEER_TOPK * PEER_TOPK)
    c_val, c_idx = lax.top_k(combo, PEER_TOPK)
    i1 = jnp.take_along_axis(i_top[:, :, 0, :], c_idx // PEER_TOPK, axis=-1)
    i2 = jnp.take_along_axis(i_top[:, :, 1, :], c_idx % PEER_TOPK, axis=-1)
    e_idx = i1 * PEER_N_KEYS + i2
    gates = jax.nn.softmax(c_val, axis=-1)
    nblk = t // PEER_TOKEN_BLOCK
    kk = PEER_HEADS * PEER_TOPK
    xb = ht.reshape(nblk, PEER_TOKEN_BLOCK, d)
    ib = e_idx.reshape(nblk, PEER_TOKEN_BLOCK, kk)
    gb = gates.reshape(nblk, PEER_TOKEN_BLOCK, kk).astype(h.dtype)

    def expert_block(args):
        x_, i_, g_ = args
        u = u_tab[i_]
        a = jax.nn.gelu(jnp.einsum('td,tkd->tk', x_, u), approximate=False)
        return jnp.einsum('tk,tkd->td', a * g_, v_tab[i_])

    return lax.map(expert_block, (xb, ib, gb)).reshape(b, s, d)


def setup_inputs(seed: int = 0) -> dict:
    key = jax.random.key(seed)
    ks = jax.random.split(key, 20)
    f32 = jnp.float32
    nrm = lambda k, shape, scale: jax.random.normal(k, shape, f32) * scale
    gain = lambda k, shape: 1.0 + 0.02 * jax.random.normal(k, shape, f32)
    x = nrm(ks[0], (BATCH, SEQ, D_MODEL), 1.0)
    mem = nrm(ks[1], (BATCH, MEM_TOKENS, D_MODEL), 1.0)
    offset = jax.random.randint(ks[2], (BATCH, 1), 0, 4096, dtype=jnp.int32)
    positions = offset + jnp.arange(SEQ, dtype=jnp.int32)[None, :]
    return {
        'x': x,
        'mem': mem,
        'positions': positions,
        'g_mix': gain(ks[3], (DEPTH, D_MODEL)),
        'w_in': nrm(ks[4], (DEPTH, D_MODEL, IN_WIDTH), D_MODEL ** -0.5),
        'w_br_ret': nrm(ks[5], (DEPTH, RET_V_W, D_MODEL), RET_V_W ** -0.5),
        'w_br_dil': nrm(ks[6], (DEPTH, DIL_OUT_W, D_MODEL), DIL_OUT_W ** -0.5),
        'w_out': nrm(ks[7], (DEPTH, D_MODEL, D_MODEL), D_MODEL ** -0.5),
        'g_cross': gain(ks[8], (DEPTH, D_MODEL)),
        'g_mem': gain(ks[9], (DEPTH, D_MODEL)),
        'w_q_mem': nrm(ks[10], (DEPTH, D_MODEL, D_MODEL), D_MODEL ** -0.5),
        'w_kv_mem': nrm(ks[11], (DEPTH, D_MODEL, 2 * D_MODEL), D_MODEL ** -0.5),
        'w_o_mem': nrm(ks[12], (DEPTH, D_MODEL, D_MODEL), D_MODEL ** -0.5),
        'g_ffn': gain(ks[13], (DEPTH, D_MODEL)),
        'w_peer_q': nrm(ks[14], (DEPTH, D_MODEL, PEER_HEADS * PEER_QUERY_DIM), D_MODEL ** -0.5),
        'peer_subkeys': nrm(ks[15], (DEPTH, PEER_HEADS, 2, PEER_N_KEYS, PEER_QUERY_DIM // 2), (PEER_QUERY_DIM // 2) ** -0.5),
        'peer_u': nrm(ks[16], (DEPTH, PEER_N_EXPERTS, D_MODEL), D_MODEL ** -0.5),
        'peer_v': nrm(ks[17], (DEPTH, PEER_N_EXPERTS, D_MODEL), PEER_HEADS ** -0.5),
        'g_final': gain(ks[18], (D_MODEL,)),
    }


def reference(x, mem, positions, g_mix, w_in, w_br_ret, w_br_dil, w_out, g_cross, g_mem, w_q_mem, w_kv_mem, w_o_mem, g_ffn, w_peer_q, peer_subkeys, peer_u, peer_v, g_final):
    b, s, d = x.shape
    for l in range(DEPTH):
        h = rms_norm(x, g_mix[l])
        proj = h @ w_in[l]
        rq, rk, rv, rg, dq, dk, dv, gate_ret, gate_dil = jnp.split(proj, IN_SPLIT_POINTS, axis=-1)
        ret = retention(rq.reshape(b, s, RET_HEADS, RET_QK_DIM), rk.reshape(b, s, RET_HEADS, RET_QK_DIM),
                        rv.reshape(b, s, RET_HEADS, RET_V_DIM), positions)
        ret = (ret.reshape(b, s, RET_V_W) * jax.nn.silu(rg.astype(jnp.float32))).astype(x.dtype)
        nh = DIL_N_GROUPS * DIL_HEADS_PER_GROUP
        dq = rotary(dq.reshape(b, s, nh, DIL_HEAD_DIM), positions)
        dk = rotary(dk.reshape(b, s, nh, DIL_HEAD_DIM), positions)
        dv = dv.reshape(b, s, nh, DIL_HEAD_DIM)
        dil = dilated_attention(dq, dk, dv).reshape(b, s, DIL_OUT_W).astype(x.dtype)
        merged = jax.nn.sigmoid(gate_ret) * (ret @ w_br_ret[l]) + jax.nn.sigmoid(gate_dil) * (dil @ w_br_dil[l])
        x = x + merged @ w_out[l]
        hc = rms_norm(x, g_cross[l])
        memn = rms_norm(mem, g_mem[l])
        x = x + memory_cross_attention(hc, memn, w_q_mem[l], w_kv_mem[l], w_o_mem[l])
        hf = rms_norm(x, g_ffn[l])
        x = x + peer_ffn(hf, w_peer_q[l], peer_subkeys[l], peer_u[l], peer_v[l])
    return rms_norm(x, g_final)
```

```python
import math
import numpy as np
from contextlib import ExitStack
import concourse.bass as bass
import concourse.mybir as mybir
from concourse.bass_utils import run_bass_kernel_spmd

F32 = mybir.dt.float32
BF16 = mybir.dt.bfloat16
I32 = mybir.dt.int32
U32 = mybir.dt.uint32
AF = mybir.ActivationFunctionType
ALU = mybir.AluOpType
AX = mybir.AxisListType

S = 2048
D = 2048
NT = 16
NCH = 16
EPS = 1e-6
WB = 256
NEG = -30000.0
TWO_PI = 2.0 * math.pi
CW1 = 6.28125
CW2 = TWO_PI - CW1

C_RQ, C_RK, C_RV, C_RG = 0, 1024, 2048, 3072
C_DQ, C_DK, C_DV = 4096, 5632, 7168
C_GR, C_GD = 8704, 10752

SAME_ENG_SYNC = True


class Buf:
    __slots__ = ("name", "w", "r")

    def __init__(self, name=""):
        self.name = name
        self.w = None
        self.r = {}


class Prog:
    COMPUTE = ("pe", "dve", "act", "pool")

    def __init__(self, nc, st, n_dma_sems=48):
        self.nc = nc
        self.q = {e: [] for e in ("pe", "dve", "act", "pool", "sp")}
        self.sem = {}
        for e in self.COMPUTE:
            self.sem[e] = st.enter_context(nc.semaphore("c_" + e))
        self.cnt = {e: 0 for e in self.COMPUTE}
        self.ndma = n_dma_sems
        for i in range(n_dma_sems):
            self.sem[("d", i)] = st.enter_context(nc.semaphore("d%d" % i))
        self.dma_uses = [0] * n_dma_sems
        self.dma_next = 0
        self.seen = {e: {} for e in self.q}
        self.n_ops = 0

    def _need(self, eng, dep, waits):
        if dep is None:
            return
        key, val, deng = dep
        if key == eng and not (SAME_ENG_SYNC and eng in ("dve", "act", "pool")):
            return
        if key == "pe" and eng == "pe":
            return
        if self.seen[eng].get(key, 0) >= val:
            return
        self.seen[eng][key] = val
        waits.append((key, val))

    def _deps(self, eng, reads, writes):
        waits = []
        for b in reads:
            self._need(eng, b.w, waits)
        for b in writes:
            self._need(eng, b.w, waits)
            for k, (v, e) in b.r.items():
                if k == eng and eng in self.COMPUTE:
                    continue
                self._need(eng, (k, v, e), waits)
        return waits

    def _commit(self, dep, reads, writes):
        for b in reads:
            cur = b.r.get(dep[0])
            if cur is None or cur[0] < dep[1]:
                b.r[dep[0]] = (dep[1], dep[2])
        for b in writes:
            b.w = dep
            b.r = {}

    def op(self, eng, fn, reads=(), writes=()):
        waits = self._deps(eng, reads, writes)
        self.cnt[eng] += 1
        dep = (eng, self.cnt[eng], eng)
        self.q[eng].append((waits, fn, (eng, 1)))
        self._commit(dep, reads, writes)
        self.n_ops += 1
        return dep

    def dma(self, qeng, fn, reads=(), writes=()):
        waits = self._deps(qeng, reads, writes)
        i = self.dma_next
        self.dma_next = (self.dma_next + 1) % self.ndma
        key = ("d", i)
        prev = 16 * self.dma_uses[i]
        if prev > 0:
            self._need(qeng, (key, prev, qeng), waits)
        self.dma_uses[i] += 1
        dep = (key, prev + 16, qeng)
        self.q[qeng].append((waits, fn, (key, 16)))
        self._commit(dep, reads, writes)
        self.n_ops += 1
        return dep

    def barrier(self):
        for eng in self.q:
            waits = []
            for e in self.COMPUTE:
                if self.cnt[e] > 0:
                    self._need(eng, (e, self.cnt[e], e), waits) if e != eng else None
            for i in range(self.ndma):
                if self.dma_uses[i] > 0:
                    self._need(eng, (("d", i), 16 * self.dma_uses[i], "x"), waits)
            self.q[eng].append((waits, None, None))

    def wait_all(self, eng, bufs):
        waits = []
        for b in bufs:
            self._need(eng, b.w, waits)
        self.q[eng].append((waits, None, None))

    def emit(self, block):
        handles = {"pe": block.tensor, "dve": block.vector, "act": block.scalar,
                   "pool": block.gpsimd, "sp": block.sync}
        sem = self.sem
        for e, dec in handles.items():
            items = self.q[e]

            def body(engh, items=items):
                for waits, fn, inc in items:
                    for k, v in waits:
                        engh.wait_ge(sem[k], v)
                    if fn is not None:
                        ins = fn(engh)
                        ins.then_inc(sem[inc[0]], inc[1])
            dec(body)


class Ring:
    def __init__(self, tiles, name="ring"):
        self.tiles = tiles
        self.bufs = [Buf("%s%d" % (name, i)) for i in range(len(tiles))]
        self.i = 0

    def next(self):
        t, b = self.tiles[self.i], self.bufs[self.i]
        self.i = (self.i + 1) % len(self.tiles)
        return t, b


def run_interleaved(gens):
    gens = list(gens)
    while gens:
        for g in list(gens):
            try:
                next(g)
            except StopIteration:
                gens.remove(g)


class Arena:
    def __init__(self, t, nbytes):
        self.t, self.n, self.off = t, nbytes, 0

    def alloc(self, shape, dty):
        esz = 2 if dty == BF16 else 4
        n = int(np.prod(shape[1:])) * esz
        n_al = (n + 63) // 64 * 64
        assert self.off + n_al <= self.n, ("arena overflow", self.off, n_al, self.n)
        v = self.t[:, self.off // 2:(self.off + n) // 2]
        if dty != BF16:
            v = v.bitcast(dty)
        self.off += n_al
        if len(shape) == 3:
            v = v.rearrange("p (a b) -> p a b", a=shape[1])
        return v

    def reset(self):
        self.off = 0


def build(stage=99, debug=False):
    nc = bass.Bass("TRN2", target_bir_lowering=False)

    def dt(name, shape, dty=F32, kind="ExternalInput"):
        return nc.dram_tensor(name, list(shape), dty, kind=kind).ap()

    skind = "ExternalOutput" if debug else "Internal"
    x = dt("x", [S, D])
    mem = dt("mem", [256, D])
    pos = dt("pos", [1, S], I32)
    g_mix = dt("g_mix", [1, D])
    w_in = dt("w_in", [D, 12800])
    w_br_ret = dt("w_br_ret", [1024, D])
    w_br_dil = dt("w_br_dil", [512, D])
    w_out = dt("w_out", [D, D])
    g_cross = dt("g_cross", [1, D])
    g_mem = dt("g_mem", [1, D])
    w_q_mem = dt("w_q_mem", [D, D])
    w_kv_mem = dt("w_kv_mem", [D, 2 * D])
    w_o_mem = dt("w_o_mem", [D, D])
    g_ffn = dt("g_ffn", [1, D])
    w_peer_q = dt("w_peer_q", [D, D])
    skT = dt("skT", [128, 16 * 128])
    peer_u = dt("peer_u", [16384, D])
    peer_v = dt("peer_v", [16384, D])
    g_final = dt("g_final", [1, D])
    cst = dt("cst", [128, CST_W])
    out = dt("out", [S, D], kind="ExternalOutput")

    ret_scr = dt("ret_scr", [8, 128, S], BF16, kind=skind)
    dil_scr = dt("dil_scr", [4, 128, S], BF16, kind=skind)
    mrg_scr = dt("mrg_scr", [16, 128, S], BF16, kind=skind)
    x2_scr = dt("x2_scr", [S, D], F32, kind=skind)
    x3_scr = dt("x3_scr", [S, D], F32, kind=skind)
    hf_scr = dt("hf_scr", [S, D], BF16, kind=skind)
    uv16 = dt("uv16", [16384, 2 * D], BF16, kind="Internal")
    dbg = {}
    if debug:
        dbg["dbg_e"] = dt("dbg_e", [S, 128], I32, kind="ExternalOutput")
        dbg["dbg_g"] = dt("dbg_g", [S, 128], F32, kind="ExternalOutput")
        dbg["dbg_a"] = dt("dbg_a", [S, 128], F32, kind="ExternalOutput")

    with ExitStack() as st:
        P = Prog(nc, st)

        def sb(name, shape, dty=F32):
            return st.enter_context(nc.sbuf_tensor(name, list(shape), dty))

        def ps(name, shape, dty=F32):
            return st.enter_context(nc.psum_tensor(name, list(shape), dty))

        def mm(o, lhsT, rhs, start, stop, reads, writes):
            P.op("pe", lambda e: e.matmul(o, lhsT, rhs, start=start, stop=stop), reads, writes)

        def tr(o, in_, ident, reads, writes):
            P.op("pe", lambda e: e.transpose(out=o, in_=in_, identity=ident), reads, writes)

        def act(o, in_, func, reads, writes, **kw):
            P.op("act", lambda e: e.activation(out=o, in_=in_, func=func, **kw), reads, writes)

        def tt(eng, o, a, b, op, reads, writes):
            P.op(eng, lambda e: e.tensor_tensor(out=o, in0=a, in1=b, op=op), reads, writes)

        def ts(eng, o, a, s1, s2, op0, op1, reads, writes):
            if op1 is None:
                P.op(eng, lambda e: e.tensor_scalar(out=o, in0=a, scalar1=s1, scalar2=None, op0=op0), reads, writes)
            else:
                P.op(eng, lambda e: e.tensor_scalar(out=o, in0=a, scalar1=s1, scalar2=s2, op0=op0, op1=op1), reads, writes)

        def stt(o, a, sc, b, op0, op1, reads, writes, accum=None):
            if accum is None:
                P.op("dve", lambda e: e.scalar_tensor_tensor(out=o, in0=a, scalar=sc, in1=b, op0=op0, op1=op1), reads, writes)
            else:
                P.op("dve", lambda e: e.scalar_tensor_tensor(out=o, in0=a, scalar=sc, in1=b, op0=op0, op1=op1, accum_out=accum), reads, writes)

        def cp(eng, o, in_, reads, writes):
            if eng == "act":
                act(o, in_, AF.Copy, reads, writes)
            else:
                P.op(eng, lambda e: e.tensor_copy(out=o, in_=in_), reads, writes)

        def dma(q, o, in_, reads, writes):
            P.dma(q, lambda e: e.dma_start(out=o, in_=in_), reads, writes)

        A1t = sb("A1", [128, 32768], BF16)
        A2t = sb("A2", [128, 32768], BF16)
        ACt = sb("AC", [128, 18432], BF16)
        A2 = Arena(A2t, 65536)
        AC = Arena(ACt, 36864)
        R1 = A1t[:, :].rearrange("p (a b) -> p a b", a=NCH)
        wring = Ring([sb("w%d" % i, [128, NCH, WB], BF16) for i in range(4)], "w")
        cst_sb = sb("cst_sb", [128, CST_W], F32)
        b_cst = Buf("cst")
        dma("sp", cst_sb[:], cst[:, :], [], [b_cst])
        ident_f = cst_sb[:, CO_IDENT:CO_IDENT + 128]
        mask01 = cst_sb[:, CO_MASK01:CO_MASK01 + 128]
        ks_c = cst_sb[:, CO_KS:CO_KS + 8]
        qs_c = cst_sb[:, CO_QS:CO_QS + 8]
        freq_c = cst_sb[:, CO_FREQ:CO_FREQ + 2]
        cb16 = sb("cb16", [128, 6 * 128], BF16)
        b_cb16 = Buf("cb16")
        cp("dve", cb16[:], cst_sb[:, CO_B16:CO_B16 + 6 * 128], [b_cst], [b_cb16])
        ident_b = cb16[:, 0:128]
        ones_b = cb16[:, 128:256]
        perm_b = [cb16[:, 256:384], cb16[:, 384:512]]
        dmask_b = [cb16[:, 512:640], cb16[:, 640:768]]
        stat = sb("stat", [128, 64], F32)

        pjring = Ring([ps("pj%d" % i, [128, 512], F32) for i in range(2)], "pj")
        tpring = Ring([ps("tp%d" % i, [128, 1024], BF16) for i in range(2)], "tp")
        mring = Ring([ps("m%d" % i, [128, 512], F32) for i in range(4)], "m")

        CONV_ROWS = 1024
        conv_todo = [(c0_, src_, r0) for r0 in range(0, 16384, CONV_ROWS) for (c0_, src_) in ((0, peer_u), (D, peer_v))]
        b_conv = []
        conv_state = {"n": 0}

        def conv_step():
            if stage >= 7 and conv_todo:
                c0_, src_, r0 = conv_todo.pop(0)
                b = Buf("conv")
                dma("pool", uv16[r0:r0 + CONV_ROWS, c0_:c0_ + D], src_[r0:r0 + CONV_ROWS, :], [], [b])
                b_conv.append(b)

        def load_w(wd, r0, nch, c0, ncols=WB):
            t, b = wring.next()
            src = wd[r0:r0 + nch * 128, c0:c0 + ncols].rearrange("(ch p) c -> p ch c", p=128)
            dma("pool", t[:, 0:nch, 0:ncols], src, [], [b])
            conv_state["n"] += 1
            if conv_state["n"] % 2 == 0:
                conv_step()
            return t, b

        def norm_pass(src, ntiles, g_dram, dstT, dst_bufs, arena, tm_out=None, src_bufs=None, b_tm=None, xarena=None):
            gb = arena.alloc([128, D], F32)
            b_gb = Buf("gb")
            dma("sp", gb, g_dram[0:1, :].partition_broadcast(128), [], [b_gb])
            xr = Ring([(xarena or arena).alloc([128, D], F32) for _ in range(4 if xarena is not None else 2)], "xt")
            xnr = Ring([arena.alloc([128, D], BF16) for _ in range(2)], "xn")
            junk = arena.alloc([128, D], BF16)
            b_junk = Buf("junk")
            def chain(c):
                for i in range(c, ntiles, 2):
                    xt, b_xt = xr.next()
                    xn, b_xn = xnr.next()
                    b_st = Buf("st")
                    dma("sp", xt, src[i * 128:(i + 1) * 128, :], [src_bufs[i]] if src_bufs is not None else [], [b_xt])
                    ssq = stat[:, 2 * (i % 16):2 * (i % 16) + 1]
                    rstd = stat[:, 2 * (i % 16) + 1:2 * (i % 16) + 2]
                    yield
                    act(junk, xt, AF.Square, [b_xt], [b_junk, b_st], accum_out=ssq)
                    yield
                    act(ssq, ssq, AF.Sqrt, [b_st], [b_st], scale=1.0 / D, bias=EPS)
                    yield
                    P.op("dve", lambda e, ssq=ssq, rstd=rstd: e.reciprocal(out=rstd, in_=ssq), [b_st], [b_st])
                    yield
                    stt(xn, xt, rstd, gb, ALU.mult, ALU.mult, [b_xt, b_st, b_gb], [b_xn])
                    yield
                    if tm_out is not None:
                        dma("pool", tm_out[i * 128:(i + 1) * 128, :], xn, [b_xn], [b_tm])
                    for half in range(2):
                        tp, b_tp = tpring.next()
                        tpv = tp[:, :].rearrange("p (a b) -> p a b", a=8)
                        for c8 in range(8):
                            ch = half * 8 + c8
                            tr(tpv[:, c8, :], xn[:, ch * 128:(ch + 1) * 128], ident_b, [b_xn, b_cb16], [b_tp])
                        yield
                        cp("act" if half == 0 else "dve", dstT[:, half * 8:half * 8 + 8, i * 128:(i + 1) * 128], tpv,
                           [b_tp], [dst_bufs[i]])
                        yield

            run_interleaved([chain(0), chain(1)])

        hT = R1
        b_hT = [Buf("hT%d" % i) for i in range(NT)]
        norm_pass(x, NT, g_mix, hT, b_hT, AC, xarena=A2)
        P.barrier()
        A2.reset()
        AC.reset()

        outbufs = []
        b_retscr = [Buf("retscr%d" % i) for i in range(8)]
        b_dilscr = [Buf("dilscr%d" % i) for i in range(4)]
        b_mrgscr = [Buf("mrgscr%d" % i) for i in range(16)]

        def make_tables(tmp, fcol, cos_t, sin_t, b_tab):
            posi = tmp.alloc([128, S], I32)
            tA = tmp.alloc([128, S], F32)
            tB = tmp.alloc([128, S], F32)
            tC = tmp.alloc([128, S], F32)
            tBi = tB.bitcast(I32)
            b_p, b_t = Buf("posi"), Buf("tt")
            dma("sp", posi, pos[0:1, :].partition_broadcast(128), [], [b_p])
            cp("dve", tC, posi, [b_p], [b_t])
            ts("dve", tA, tC, fcol, None, ALU.mult, None, [b_t, b_cst], [b_t])
            ts("dve", tBi, tA, 1.0 / TWO_PI, None, ALU.mult, None, [b_t], [b_t])
            cp("dve", tC, tBi, [b_t], [b_t])
            stt(tA, tC, -CW1, tA, ALU.mult, ALU.add, [b_t], [b_t])
            stt(tA, tC, -CW2, tA, ALU.mult, ALU.add, [b_t], [b_t])
            ts("dve", tC, tA, math.pi, -TWO_PI, ALU.is_gt, ALU.mult, [b_t], [b_t])
            tt("dve", tB, tA, tC, ALU.add, [b_t], [b_t])
            act(sin_t, tB, AF.Sin, [b_t], [b_tab])
            ts("dve", tA, tA, math.pi / 2, None, ALU.add, None, [b_t], [b_t])
            ts("dve", tC, tA, math.pi, -TWO_PI, ALU.is_gt, ALU.mult, [b_t], [b_t])
            tt("dve", tB, tA, tC, ALU.add, [b_t], [b_t])
            act(cos_t, tB, AF.Sin, [b_t], [b_tab])

        def proj_rot(wt, b_w, wc0, raw_ring, cos_t, sin_t, b_tab, perm, dst, b_dst, t1r, t2r):
            raw, b_raw = raw_ring.next()
            for tb in range(4):
                pj, b_pj = pjring.next()
                for ch in range(NCH):
                    mm(pj[:, :], wt[:, ch, wc0:wc0 + 128], hT[:, ch, tb * 512:(tb + 1) * 512], ch == 0, ch == NCH - 1,
                       [b_w] + b_hT[tb * 4:tb * 4 + 4], [b_pj])
                cp("act", raw[:, tb * 512:(tb + 1) * 512], pj[:, :], [b_pj], [b_raw])
            for tb in range(4):
                sl = slice(tb * 512, (tb + 1) * 512)
                m, b_m = mring.next()
                mm(m[:, :], perm, raw[:, sl], True, True, [b_raw, b_cb16], [b_m])
                t1, b_t1 = t1r.next()
                t2, b_t2 = t2r.next()
                tt("pool", t1, raw[:, sl], cos_t[:, sl], ALU.mult, [b_raw, b_tab], [b_t1])
                tt("dve", t2, m[:, :], sin_t[:, sl], ALU.mult, [b_m, b_tab], [b_t2])
                tt("dve", dst[:, sl], t1, t2, ALU.add, [b_t1, b_t2], [b_dst])

        if stage >= 2:
            cos_r = A2.alloc([128, S], F32)
            sin_r = A2.alloc([128, S], F32)
            b_tabr = Buf("tabr")
            make_tables(AC, freq_c[:, 0:1], cos_r, sin_r, b_tabr)
            P.barrier()
            AC.reset()
            raw_ring = Ring([A2.alloc([128, S], BF16) for _ in range(2)], "raw")
            qT = [A2.alloc([128, S], BF16) for _ in range(2)]
            kT = [A2.alloc([128, S], BF16) for _ in range(2)]
            b_qT = [Buf("qT0"), Buf("qT1")]
            b_kT = [Buf("kT0"), Buf("kT1")]
            v_sb = A2.alloc([128, NT, 256], BF16)
            rgs = A2.alloc([128, NT, 256], BF16)
            b_v = [Buf("v%d" % i) for i in range(NT)]
            b_rg = [Buf("rg%d" % i) for i in range(NT)]
            t1r = Ring([AC.alloc([128, 512], F32) for _ in range(2)], "t1")
            t2r = Ring([AC.alloc([128, 512], F32) for _ in range(2)], "t2")
            retT_ring = Ring([AC.alloc([128, S], BF16) for _ in range(2)], "retT")
            U = [AC.alloc([128, 128], F32) for _ in range(2)]
            b_U = [Buf("U0"), Buf("U1")]
            Sb_ring = [Ring([AC.alloc([128, 128], BF16) for _ in range(2)], "Sb%d" % i) for i in range(2)]
            kp_ring = Ring([AC.alloc([128, 128], BF16) for _ in range(3)], "kp")
            sc_ring = Ring([AC.alloc([128, 128], BF16) for _ in range(3)], "sc")
            rt_ring = Ring([AC.alloc([128, 128], BF16) for _ in range(3)], "rt")
            junk_s = AC.alloc([128, 128], BF16)
            b_junk_s = Buf("junk_s")
            st_ring = Ring([stat[:, 32 + 4 * i:32 + 4 * i + 4] for i in range(8)], "rst")

            for pr in range(4):
                h0 = 2 * pr
                wq, b_wq = load_w(w_in, 0, NCH, C_RQ + h0 * 128)
                wk, b_wk = load_w(w_in, 0, NCH, C_RK + h0 * 128)
                wv, b_wv = load_w(w_in, 0, NCH, C_RV + h0 * 128)
                wg, b_wg = load_w(w_in, 0, NCH, C_RG + h0 * 128)
                for hh in range(2):
                    proj_rot(wq, b_wq, hh * 128, raw_ring, cos_r, sin_r, b_tabr, perm_b[0], qT[hh], b_qT[hh], t1r, t2r)
                    proj_rot(wk, b_wk, hh * 128, raw_ring, cos_r, sin_r, b_tabr, perm_b[0], kT[hh], b_kT[hh], t1r, t2r)
                for i in range(NT):
                    pj, b_pj = pjring.next()
                    for ch in range(NCH):
                        mm(pj[:, 0:256], hT[:, ch, i * 128:(i + 1) * 128], wv[:, ch, :], ch == 0, ch == NCH - 1,
                           [b_wv, b_hT[i]], [b_pj])
                    cp("act", v_sb[:, i, :], pj[:, 0:256], [b_pj], [b_v[i]])
                    pj, b_pj = pjring.next()
                    for ch in range(NCH):
                        mm(pj[:, 0:256], hT[:, ch, i * 128:(i + 1) * 128], wg[:, ch, :], ch == 0, ch == NCH - 1,
                           [b_wg, b_hT[i]], [b_pj])
                    act(rgs[:, i, :], pj[:, 0:256], AF.Silu, [b_pj], [b_rg[i]])
                retT = []
                for hh in range(2):
                    retT.append(retT_ring.next())
                def ret_chain(hh):
                    h = h0 + hh
                    Sb_prev = None
                    for n in range(NT):
                        csl = slice(n * 128, (n + 1) * 128)
                        vsl = v_sb[:, n, hh * 128:(hh + 1) * 128]
                        tp, b_tp = tpring.next()
                        tr(tp[:, 0:128], kT[hh][:, csl], ident_b, [b_kT[hh], b_cb16], [b_tp])
                        m, b_m = mring.next()
                        mm(m[:, 0:128], kT[hh][:, csl], qT[hh][:, csl], True, True, [b_kT[hh], b_qT[hh]], [b_m])
                        yield
                        kp, b_kp = kp_ring.next()
                        act(kp, tp[:, 0:128], AF.Copy, [b_tp, b_cst], [b_kp], scale=ks_c[:, h:h + 1])
                        sc, b_sc = sc_ring.next()
                        stt(sc, m[:, 0:128], ks_c[:, h:h + 1], mask01, ALU.mult, ALU.mult, [b_m, b_cst], [b_sc])
                        yield
                        mo, b_mo = mring.next()
                        mm(mo[:, 0:128], sc, vsl, True, n == 0, [b_sc, b_v[n]], [b_mo])
                        if n > 0:
                            mm(mo[:, 0:128], qT[hh][:, csl], Sb_prev[0], False, True, [b_qT[hh], Sb_prev[1]], [b_mo])
                        mk, b_mk = mring.next()
                        mm(mk[:, 0:128], kp, vsl, True, True, [b_kp, b_v[n]], [b_mk])
                        yield
                        if n == 0:
                            cp("dve", U[hh], mk[:, 0:128], [b_mk], [b_U[hh]])
                        else:
                            stt(U[hh], U[hh], GAMMA_C[h], mk[:, 0:128], ALU.mult, ALU.add, [b_mk, b_U[hh]], [b_U[hh]])
                        stq, b_stq = st_ring.next()
                        act(junk_s, mo[:, 0:128], AF.Square, [b_mo, b_cst], [b_junk_s, b_stq], scale=qs_c[:, h:h + 1],
                            accum_out=stq[:, 0:1])
                        yield
                        if n < NT - 1:
                            Sb, b_Sb = Sb_ring[hh].next()
                            act(Sb, U[hh], AF.Copy, [b_U[hh]], [b_Sb], scale=GAMMA_C[h])
                            Sb_prev = (Sb, b_Sb)
                        act(stq[:, 1:2], stq[:, 0:1], AF.Sqrt, [b_stq], [b_stq], scale=1.0 / 128, bias=EPS)
                        yield
                        P.op("dve", lambda e, stq=stq: e.reciprocal(out=stq[:, 2:3], in_=stq[:, 1:2]), [b_stq], [b_stq])
                        yield
                        tt("dve", stq[:, 3:4], stq[:, 2:3], qs_c[:, h:h + 1], ALU.mult, [b_stq, b_cst], [b_stq])
                        yield
                        rt, b_rt = rt_ring.next()
                        stt(rt, mo[:, 0:128], stq[:, 3:4], rgs[:, n, hh * 128:(hh + 1) * 128], ALU.mult, ALU.mult,
                            [b_mo, b_stq, b_rg[n]], [b_rt])
                        yield
                        tp, b_tp = tpring.next()
                        tr(tp[:, 0:128], rt, ident_b, [b_rt, b_cb16], [b_tp])
                        yield
                        cp("act", retT[hh][0][:, csl], tp[:, 0:128], [b_tp], [retT[hh][1]])
                        yield

                run_interleaved([ret_chain(0), ret_chain(1)])
                for hh in range(2):
                    dma("sp", ret_scr[h0 + hh, :, :], retT[hh][0], [retT[hh][1]], [b_retscr[h0 + hh]])
                    outbufs.append(b_retscr[h0 + hh])
            P.barrier()
            A2.reset()
            AC.reset()

        if stage >= 3:
            cos_d = A2.alloc([128, S], F32)
            sin_d = A2.alloc([128, S], F32)
            b_tabd = Buf("tabd")
            make_tables(AC, freq_c[:, 1:2], cos_d, sin_d, b_tabd)
            P.barrier()
            AC.reset()
            raw_ring = Ring([A2.alloc([128, S], BF16) for _ in range(2)], "raw")
            qT = [A2.alloc([128, S], BF16) for _ in range(2)]
            kT = [A2.alloc([128, S], BF16) for _ in range(2)]
            b_qT = [Buf("dqT0"), Buf("dqT1")]
            b_kT = [Buf("dkT0"), Buf("dkT1")]
            vblk = A2.alloc([128, 16, 256], BF16)
            b_vb = [Buf("vb%d" % i) for i in range(16)]
            t1r = Ring([A2.alloc([128, 512], F32) for _ in range(2)], "t1")
            t2r = Ring([A2.alloc([128, 512], F32) for _ in range(2)], "t2")
            pT_ring = Ring([A2.alloc([128, 256], BF16) for _ in range(3)], "pT")
            acc_n = [AC.alloc([128, S], F32) for _ in range(2)]
            acc_d = [AC.alloc([128, S], F32) for _ in range(2)]
            b_an = [Buf("an0"), Buf("an1")]
            b_ad = [Buf("ad0"), Buf("ad1")]
            DSCALE = 128.0 ** -0.5

            def blk_cols(g, bi):
                if g == 0:
                    return slice(bi * 128, (bi + 1) * 128)
                if g == 1:
                    c, n = bi // 4, bi % 4
                    s0 = c + n * 128 * 4
                    return slice(s0, s0 + 127 * 4 + 1, 4)
                return slice(bi, bi + 127 * 16 + 1, 16)

            def has_prev(g, bi):
                return (g == 0 and bi > 0) or (g == 1 and bi % 4 > 0)

            def acc_view(acc, g, k):
                if g == 0:
                    return acc[:, k * 512:(k + 1) * 512]
                if g == 1:
                    return acc[:, k:k + 4 * 511 + 1:4]
                return acc.rearrange("p (i c) -> p c i", c=16)[:, 4 * k:4 * k + 4, :]

            for jp in range(2):
                for g in range(3):
                    hd0 = g * 4 + 2 * jp
                    wq, b_wq = load_w(w_in, 0, NCH, C_DQ + hd0 * 128)
                    wk, b_wk = load_w(w_in, 0, NCH, C_DK + hd0 * 128)
                    wv, b_wv = load_w(w_in, 0, NCH, C_DV + hd0 * 128)
                    for hh in range(2):
                        proj_rot(wq, b_wq, hh * 128, raw_ring, cos_d, sin_d, b_tabd, perm_b[1], qT[hh], b_qT[hh], t1r, t2r)
                        proj_rot(wk, b_wk, hh * 128, raw_ring, cos_d, sin_d, b_tabd, perm_b[1], kT[hh], b_kT[hh], t1r, t2r)
                    for bi in range(16):
                        cols = blk_cols(g, bi)
                        pj, b_pj = pjring.next()
                        for ch in range(NCH):
                            mm(pj[:, 0:256], hT[:, ch, cols], wv[:, ch, :], ch == 0, ch == NCH - 1, [b_wv] + b_hT, [b_pj])
                        cp("act", vblk[:, bi, :], pj[:, 0:256], [b_pj], [b_vb[bi]])
                    for k in range(4):
                        for hh in range(2):
                            mn, b_mn = mring.next()
                            md, b_md = mring.next()
                            for blk in range(4):
                                bi = 4 * k + blk
                                qc = blk_cols(g, bi)
                                hp = has_prev(g, bi)
                                sc, b_sc = pjring.next()
                                if hp:
                                    pc = blk_cols(g, bi - 1)
                                    mm(sc[:, 0:128], kT[hh][:, pc], qT[hh][:, qc], True, False, [b_kT[hh], b_qT[hh]], [b_sc])
                                    mm(sc[:, 0:128], ident_b, dmask_b[0], False, True, [b_cb16], [b_sc])
                                mm(sc[:, 128:256], kT[hh][:, qc], qT[hh][:, qc], True, False, [b_kT[hh], b_qT[hh]], [b_sc])
                                mm(sc[:, 128:256], ident_b, dmask_b[1], False, True, [b_cb16], [b_sc])
                                pT, b_pT = pT_ring.next()
                                lo = 0 if hp else 128
                                act(pT[:, lo:256], sc[:, lo:256], AF.Exp, [b_sc], [b_pT], scale=DSCALE)
                                osl = slice(blk * 128, (blk + 1) * 128)
                                vs = slice(hh * 128, (hh + 1) * 128)
                                if hp:
                                    mm(mn[:, osl], vblk[:, bi - 1, vs], pT[:, 0:128], True, False, [b_vb[bi - 1], b_pT], [b_mn])
                                    mm(md[:, osl], ones_b, pT[:, 0:128], True, False, [b_cb16, b_pT], [b_md])
                                mm(mn[:, osl], vblk[:, bi, vs], pT[:, 128:256], not hp, True, [b_vb[bi], b_pT], [b_mn])
                                mm(md[:, osl], ones_b, pT[:, 128:256], not hp, True, [b_cb16, b_pT], [b_md])
                            for (mp, b_mp, acc, b_acc) in ((mn, b_mn, acc_n[hh], b_an[hh]), (md, b_md, acc_d[hh], b_ad[hh])):
                                av = acc_view(acc, g, k)
                                mv = mp[:, :] if g < 2 else mp[:, :].rearrange("p (c i) -> p c i", c=4)
                                if g == 0:
                                    cp("act", av, mv, [b_mp], [b_acc])
                                else:
                                    tt("dve", av, mv, av, ALU.add, [b_mp, b_acc], [b_acc])
                for hh in range(2):
                    P.op("dve", lambda e, hh=hh: e.reciprocal(out=acc_d[hh], in_=acc_d[hh]), [b_ad[hh]], [b_ad[hh]])
                    stg, b_stg = raw_ring.next()
                    tt("dve", stg, acc_n[hh], acc_d[hh], ALU.mult, [b_an[hh], b_ad[hh]], [b_stg])
                    dma("sp", dil_scr[2 * jp + hh, :, :], stg, [b_stg], [b_dilscr[2 * jp + hh]])
                    outbufs.append(b_dilscr[2 * jp + hh])
            P.barrier()
            A2.reset()
            AC.reset()

        R2 = A2t[:, :].rearrange("p (a b) -> p a b", a=NCH)
        b_R1 = [Buf("R1_%d" % i) for i in range(NT)]
        b_R2 = [Buf("R2_%d" % i) for i in range(NT)]
        b_x2 = [Buf("x2_%d" % i) for i in range(NT)]
        b_x3 = [Buf("x3_%d" % i) for i in range(NT)]
        b_hfs = Buf("hf_scr")

        if stage >= 4:
            retT_all = A2.alloc([128, 8, S], BF16)
            dilT_all = A2.alloc([128, 4, S], BF16)
            b_rta, b_dta = Buf("rta"), Buf("dta")
            for h in range(8):
                dma("sp", retT_all[:, h, :], ret_scr[h, :, :], [b_retscr[h]], [b_rta])
            for j in range(4):
                dma("sp", dilT_all[:, j, :], dil_scr[j, :, :], [b_dilscr[j]], [b_dta])
            sg_ring = Ring([AC.alloc([128, 512], F32) for _ in range(4)], "sg")
            ta_ring = Ring([AC.alloc([128, 512], F32) for _ in range(4)], "ta")
            mst_ring = Ring([AC.alloc([128, S], BF16) for _ in range(2)], "mst")
            wbr_ring = Ring([AC.alloc([128, 8, WB], BF16) for _ in range(2)], "wbr")
            wbd_ring = Ring([AC.alloc([128, 4, WB], BF16) for _ in range(2)], "wbd")
            for mc2 in range(8):
                wgr, b_wgr = load_w(w_in, 0, NCH, C_GR + mc2 * 256)
                wgd, b_wgd = load_w(w_in, 0, NCH, C_GD + mc2 * 256)
                wbr, b_wbr = wbr_ring.next()
                dma("pool", wbr, w_br_ret[:, mc2 * 256:(mc2 + 1) * 256].rearrange("(ch p) c -> p ch c", p=128), [], [b_wbr])
                wbd, b_wbd = wbd_ring.next()
                dma("pool", wbd, w_br_dil[:, mc2 * 256:(mc2 + 1) * 256].rearrange("(ch p) c -> p ch c", p=128), [], [b_wbd])
                for sub in range(2):
                    mc = 2 * mc2 + sub
                    wsl = slice(sub * 128, (sub + 1) * 128)
                    mst, b_mst = mst_ring.next()
                    for tb in range(4):
                        tsl = slice(tb * 512, (tb + 1) * 512)
                        hb = b_hT[tb * 4:tb * 4 + 4]
                        sgs = []
                        for (wg_, b_wg_) in ((wgr, b_wgr), (wgd, b_wgd)):
                            pj, b_pj = pjring.next()
                            for ch in range(NCH):
                                mm(pj[:, :], wg_[:, ch, wsl], hT[:, ch, tsl], ch == 0, ch == NCH - 1, [b_wg_] + hb, [b_pj])
                            sg, b_sg = sg_ring.next()
                            act(sg, pj[:, :], AF.Sigmoid, [b_pj], [b_sg])
                            sgs.append((sg, b_sg))
                        m1, b_m1 = mring.next()
                        for kc in range(8):
                            mm(m1[:, :], wbr[:, kc, wsl], retT_all[:, kc, tsl], kc == 0, kc == 7, [b_wbr, b_rta], [b_m1])
                        m2, b_m2 = mring.next()
                        for kc in range(4):
                            mm(m2[:, :], wbd[:, kc, wsl], dilT_all[:, kc, tsl], kc == 0, kc == 3, [b_wbd, b_dta], [b_m2])
                        ta, b_ta = ta_ring.next()
                        tb_, b_tb = ta_ring.next()
                        tt("dve", ta, m1[:, :], sgs[0][0], ALU.mult, [b_m1, sgs[0][1]], [b_ta])
                        tt("dve", tb_, m2[:, :], sgs[1][0], ALU.mult, [b_m2, sgs[1][1]], [b_tb])
                        tt("dve", mst[:, tsl], ta, tb_, ALU.add, [b_ta, b_tb], [b_mst])
                    dma("sp", mrg_scr[mc, :, :], mst, [b_mst], [b_mrgscr[mc]])
                    outbufs.append(b_mrgscr[mc])
            P.barrier()
            A2.reset()
            AC.reset()

        def resid_proj(actT, b_act, wd, resid, b_res, dst, b_dst):
            RB = 512
            w2ring = Ring([A2.alloc([128, NCH, RB], BF16) for _ in range(4)], "w2")
            xp_ring = Ring([AC.alloc([128, RB], F32) for _ in range(4)], "xp")
            op_ring = Ring([AC.alloc([128, RB], F32) for _ in range(4)], "op")
            for cb in range(D // RB):
                w, b_w = w2ring.next()
                dma("pool", w, wd[:, cb * RB:(cb + 1) * RB].rearrange("(ch p) c -> p ch c", p=128), [], [b_w])
                for i in range(NT):
                    rs = slice(i * 128, (i + 1) * 128)
                    pj, b_pj = pjring.next()
                    for kc in range(NCH):
                        mm(pj[:, :], actT[:, kc, rs], w[:, kc, :], kc == 0, kc == NCH - 1, [b_w, b_act[i]], [b_pj])
                    xp, b_xp = xp_ring.next()
                    dma("sp", xp, resid[rs, cb * RB:(cb + 1) * RB], [b_res[i]] if b_res is not None else [], [b_xp])
                    o, b_o = op_ring.next()
                    tt("dve", o, pj[:, :], xp, ALU.add, [b_pj, b_xp], [b_o])
                    dma("act", dst[rs, cb * RB:(cb + 1) * RB], o, [b_o], [b_dst[i]])

        if stage >= 5:
            for mc in range(16):
                dma("sp", R1[:, mc, :], mrg_scr[mc, :, :], [b_mrgscr[mc]] + b_hT, b_R1)
            resid_proj(R1, b_R1, w_out, x, None, x2_scr, b_x2)
            outbufs.extend(b_x2)
            P.barrier()
            AC.reset()
            A2.reset()
            norm_pass(x2_scr, NT, g_cross, R2, b_R2, AC, src_bufs=b_x2, xarena=Arena(A1t, 65536))
            P.barrier()
            AC.reset()

        if stage >= 6:
            A1 = Arena(A1t, 65536)
            memnT = A1.alloc([128, NCH, 256], BF16)
            b_mn = [Buf("mn0"), Buf("mn1")]
            norm_pass(mem, 2, g_mem, memnT, b_mn, AC)
            P.barrier()
            AC.reset()
            kmT = AC.alloc([128, NCH, 256], BF16)
            vm = AC.alloc([128, 2, D], BF16)
            b_km, b_vm = Buf("km"), Buf("vm")
            kvring = Ring([A1.alloc([128, NCH, 512], BF16) for _ in range(3)], "kvw")
            for cb in range(D // 512):
                w, b_w = kvring.next()
                dma("pool", w, w_kv_mem[:, cb * 512:(cb + 1) * 512].rearrange("(ch p) c -> p ch c", p=128), [], [b_w])
                for sub in range(4):
                    pj, b_pj = pjring.next()
                    for ch in range(NCH):
                        mm(pj[:, 0:256], w[:, ch, sub * 128:(sub + 1) * 128], memnT[:, ch, :], ch == 0, ch == NCH - 1,
                           [b_w] + b_mn, [b_pj])
                    cp("act", kmT[:, 4 * cb + sub, :], pj[:, 0:256], [b_pj], [b_km])
            for cb in range(D // 512):
                w, b_w = kvring.next()
                dma("pool", w, w_kv_mem[:, D + cb * 512:D + (cb + 1) * 512].rearrange("(ch p) c -> p ch c", p=128), [], [b_w])
                for mt in range(2):
                    pj, b_pj = pjring.next()
                    for ch in range(NCH):
                        mm(pj[:, :], memnT[:, ch, mt * 128:(mt + 1) * 128], w[:, ch, :], ch == 0, ch == NCH - 1,
                           [b_w, b_mn[mt]], [b_pj])
                    cp("act", vm[:, mt, cb * 512:(cb + 1) * 512], pj[:, :], [b_pj], [b_vm])
            P.barrier()
            oT = R1
            b_oT = [Buf("oT%d" % i) for i in range(NT)]
            qm = AC.alloc([128, 4, S], BF16)
            b_qm = Buf("qm")
            p_ring = Ring([AC.alloc([128, 256], F32) for _ in range(2)], "p")
            pn_ring = Ring([AC.alloc([128, 256], BF16) for _ in range(2)], "pn")
            pt_ring = Ring([AC.alloc([128, 256], BF16) for _ in range(2)], "pt")
            cst_ring = Ring([stat[:, 32 + 4 * i:32 + 4 * i + 4] for i in range(8)], "cst")
            MSCALE = 512.0 ** -0.5
            for hm in range(4):
                for wb in range(2):
                    w, b_w = load_w(w_q_mem, 0, NCH, hm * 512 + wb * WB)
                    for sub in range(2):
                        for tb in range(4):
                            pj, b_pj = pjring.next()
                            for ch in range(NCH):
                                mm(pj[:, :], w[:, ch, sub * 128:(sub + 1) * 128], R2[:, ch, tb * 512:(tb + 1) * 512],
                                   ch == 0, ch == NCH - 1, [b_w] + b_R2[tb * 4:tb * 4 + 4], [b_pj])
                            cp("act", qm[:, 2 * wb + sub, tb * 512:(tb + 1) * 512], pj[:, :], [b_pj], [b_qm])
                def att_chain(c0, hm=hm):
                    for i in range(c0, NT, 2):
                        rs = slice(i * 128, (i + 1) * 128)
                        m, b_m = mring.next()
                        for c in range(4):
                            mm(m[:, 0:256], qm[:, c, rs], kmT[:, hm * 4 + c, :], c == 0, c == 3, [b_qm, b_km], [b_m])
                        yield
                        sq, b_sq = cst_ring.next()
                        P.op("dve", lambda e, m=m, sq=sq: e.reduce_max(out=sq[:, 0:1], in_=m[:, 0:256], axis=AX.X), [b_m], [b_sq])
                        yield
                        ts("dve", sq[:, 1:2], sq[:, 0:1], -MSCALE, None, ALU.mult, None, [b_sq], [b_sq])
                        yield
                        p, b_p = p_ring.next()
                        act(p, m[:, 0:256], AF.Exp, [b_m, b_sq], [b_p, b_sq], scale=MSCALE, bias=sq[:, 1:2], accum_out=sq[:, 2:3])
                        yield
                        P.op("dve", lambda e, sq=sq: e.reciprocal(out=sq[:, 3:4], in_=sq[:, 2:3]), [b_sq], [b_sq])
                        yield
                        pn, b_pn = pn_ring.next()
                        act(pn, p, AF.Copy, [b_p, b_sq], [b_pn], scale=sq[:, 3:4])
                        yield
                        tp, b_tp = tpring.next()
                        for mt in range(2):
                            tr(tp[:, mt * 128:(mt + 1) * 128], pn[:, mt * 128:(mt + 1) * 128], ident_b, [b_pn, b_cb16], [b_tp])
                        yield
                        pt, b_pt = pt_ring.next()
                        cp("act", pt, tp[:, 0:256], [b_tp], [b_pt])
                        yield
                        mo, b_mo = mring.next()
                        for c in range(4):
                            for mt in range(2):
                                mm(mo[:, c * 128:(c + 1) * 128], vm[:, mt, (hm * 4 + c) * 128:(hm * 4 + c + 1) * 128],
                                   pt[:, mt * 128:(mt + 1) * 128], mt == 0, mt == 1, [b_vm, b_pt], [b_mo])
                        yield
                        cp("dve", oT[:, hm * 4:hm * 4 + 4, rs], mo[:, :].rearrange("p (c t) -> p c t", c=4), [b_mo], [b_oT[i]])
                        yield

                run_interleaved([att_chain(0), att_chain(1)])
            P.barrier()
            AC.reset()
            A2.reset()
            resid_proj(oT, b_oT, w_o_mem, x2_scr, b_x2, x3_scr, b_x3)
            outbufs.extend(b_x3)
            P.barrier()
            AC.reset()
            A2.reset()
            norm_pass(x3_scr, NT, g_ffn, R2, b_R2, AC, src_bufs=b_x3, tm_out=hf_scr, b_tm=b_hfs, xarena=Arena(A1t, 65536))
            outbufs.append(b_hfs)
            P.barrier()
            AC.reset()

        if stage >= 7:
            qpT = R1
            b_qp = Buf("qpT")
            for cb in range(D // WB):
                w, b_w = load_w(w_peer_q, 0, NCH, cb * WB)
                for sub in range(2):
                    for tb in range(4):
                        pj, b_pj = pjring.next()
                        for ch in range(NCH):
                            mm(pj[:, :], w[:, ch, sub * 128:(sub + 1) * 128], R2[:, ch, tb * 512:(tb + 1) * 512],
                               ch == 0, ch == NCH - 1, [b_w] + b_R2[tb * 4:tb * 4 + 4], [b_pj])
                        cp("act", qpT[:, 2 * cb + sub, tb * 512:(tb + 1) * 512], pj[:, :], [b_pj], [b_qp])
            while conv_todo:
                conv_step()
            P.barrier()
            A2.reset()
            AC.reset()
            iota16 = cst_sb[:, CO_IOTA:CO_IOTA + 16]
            sk_sb = A2.alloc([128, 16, 128], BF16)
            b_sk = Buf("sk")
            dma("pool", sk_sb, skT[:, :].rearrange("p (a n) -> p a n", a=16), [], [b_sk])
            gfb = A2.alloc([128, D], F32)
            b_gfb = Buf("gfb")
            dma("sp", gfb, g_final[0:1, :].partition_broadcast(128), [], [b_gfb])
            sc_sb = A2.alloc([128, 16, 128], F32)
            combo = A2.alloc([128, 8, 256], F32)
            onehot = A2.alloc([128, 128, 16], F32)
            prod = A2.alloc([128, 128, 16], F32)
            b_sc, b_combo, b_oh, b_prod = Buf("sc"), Buf("combo"), Buf("oh"), Buf("prod")
            vtop = A2.alloc([128, 16, 16], F32)
            itop = A2.alloc([128, 16, 16], U32)
            itopf = A2.alloc([128, 16, 16], F32)
            b_vt, b_it, b_itf = Buf("vt"), Buf("it"), Buf("itf")
            work_ring = Ring([A2.alloc([128, 128], F32) for _ in range(1)], "work")
            cwork_ring = Ring([A2.alloc([128, 256], F32) for _ in range(1)], "cwork")
            cval = A2.alloc([128, 8, 16], F32)
            cidx = A2.alloc([128, 8, 16], U32)
            b_cv, b_ci = Buf("cv"), Buf("ci")
            ab_i = [A2.alloc([128, 128], U32) for _ in range(2)]
            ab_f = [A2.alloc([128, 128], F32) for _ in range(2)]
            i12f = [A2.alloc([128, 128], F32) for _ in range(2)]
            b_ab = Buf("ab")
            e_f = A2.alloc([128, 128], F32)
            ex = A2.alloc([128, 8, 16], F32)
            gsum = A2.alloc([128, 8], F32)
            b_ef, b_ex = Buf("ef"), Buf("ex")
            e_i = [A2.alloc([128, 128], I32) for _ in range(2)]
            gates = [A2.alloc([128, 8, 16], F32) for _ in range(2)]
            araw = [A2.alloc([128, 128], F32) for _ in range(2)]
            gel = [A2.alloc([128, 128], F32) for _ in range(2)]
            b_e = [Buf("e0"), Buf("e1")]
            b_g = [Buf("g0"), Buf("g1")]
            b_ar = [[Buf() for _ in range(128)] for _ in range(2)]
            b_gl = [[Buf() for _ in range(128)] for _ in range(2)]
            hf_t = AC.alloc([128, D], BF16)
            b_hft = Buf("hft")
            junkp = A2.alloc([128, D], BF16)
            b_jp = Buf("junkp")
            diag_ring = Ring([A2.alloc([128, 128], BF16) for _ in range(4)], "diag")
            x3t = AC.alloc([128, D], F32)
            b_x3t = Buf("x3t")
            wmem = [t[:, :, :].rearrange("p a b -> p (a b)") for t in wring.tiles]
            g_ring = Ring([AC.alloc([128, 2 * D], BF16) for _ in range(3)] + wmem, "gth")
            fst = stat[:, 32:36]
            b_fst = Buf("fst")
            acc_banks = [mring.next() for _ in range(4)]

            def top16(src, vout, iout, wring_, b_src, b_v, b_i):
                wk, b_wk = wring_.next()
                P.op("dve", lambda e: e.max(out=vout[:, 0:8], in_=src), [b_src], [b_v])
                P.op("dve", lambda e: e.match_replace(out=wk, in_to_replace=vout[:, 0:8], in_values=src, imm_value=-1e30),
                     [b_src, b_v], [b_wk])
                P.op("dve", lambda e: e.max(out=vout[:, 8:16], in_=wk), [b_wk], [b_v])
                P.op("dve", lambda e: e.max_index(out=iout[:, 0:8], in_max=vout[:, 0:8], in_values=src), [b_src, b_v], [b_i])
                P.op("dve", lambda e: e.max_index(out=iout[:, 8:16], in_max=vout[:, 8:16], in_values=src), [b_src, b_v], [b_i])

            def idx_phase(i):
                sl = i % 2
                rs = slice(i * 128, (i + 1) * 128)
                for grp in range(4):
                    pj, b_pj = pjring.next()
                    for q4 in range(4):
                        hp = grp * 4 + q4
                        mm(pj[:, q4 * 128:(q4 + 1) * 128], qpT[:, hp, rs], sk_sb[:, hp, :], True, True, [b_qp, b_sk], [b_pj])
                    cp("act", sc_sb[:, grp * 4:grp * 4 + 4, :], pj[:, :].rearrange("p (a n) -> p a n", a=4), [b_pj], [b_sc])
                yield
                for hp in range(16):
                    top16(sc_sb[:, hp, :], vtop[:, hp, :], itop[:, hp, :], work_ring, b_sc, b_vt, b_it)
                    if hp % 4 == 3:
                        yield
                cp("dve", itopf, itop, [b_it], [b_itf])
                vt4 = vtop.rearrange("p (h q) k -> p h q k", q=2)
                itf4 = itopf.rearrange("p (h q) k -> p h q k", q=2)
                combo4 = combo.rearrange("p h (a b) -> p h a b", a=16)
                tt("dve", combo4, vt4[:, :, 0, :].unsqueeze(3).broadcast_to([128, 8, 16, 16]),
                   vt4[:, :, 1, :].unsqueeze(2).broadcast_to([128, 8, 16, 16]), ALU.add, [b_vt], [b_combo])
                for h in range(8):
                    top16(combo[:, h, :], cval[:, h, :], cidx[:, h, :], cwork_ring, b_combo, b_cv, b_ci)
                    if h % 4 == 3:
                        yield
                cidx2 = cidx.rearrange("p h k -> p (h k)")
                ts("dve", ab_i[0], cidx2, 4, None, ALU.logical_shift_right, None, [b_ci], [b_ab])
                ts("dve", ab_i[1], cidx2, 15, None, ALU.bitwise_and, None, [b_ci], [b_ab])
                for q in range(2):
                    cp("dve", ab_f[q], ab_i[q], [b_ab], [b_ab])
                    tt("dve", onehot, ab_f[q].unsqueeze(2).broadcast_to([128, 128, 16]),
                       iota16.unsqueeze(1).broadcast_to([128, 128, 16]), ALU.is_equal, [b_ab, b_cst], [b_oh])
                    tt("dve", prod.rearrange("p (h k) j -> p h k j", h=8), onehot.rearrange("p (h k) j -> p h k j", h=8),
                       itf4[:, :, q, :].unsqueeze(2).broadcast_to([128, 8, 16, 16]), ALU.mult, [b_oh, b_itf], [b_prod])
                    P.op("dve", lambda e, q=q: e.tensor_reduce(out=i12f[q], in_=prod, axis=AX.X, op=ALU.add), [b_prod], [b_ab])
                    yield
                stt(e_f, i12f[0], 128.0, i12f[1], ALU.mult, ALU.add, [b_ab], [b_ef])
                cp("dve", e_i[sl], e_f, [b_ef], [b_e[sl]])
                tt("dve", ex, cval, cval[:, :, 0:1].broadcast_to([128, 8, 16]), ALU.subtract, [b_cv], [b_ex])
                act(ex, ex, AF.Exp, [b_ex], [b_ex])
                P.op("dve", lambda e: e.tensor_reduce(out=gsum, in_=ex, axis=AX.X, op=ALU.add), [b_ex], [b_ex])
                P.op("dve", lambda e: e.reciprocal(out=gsum, in_=gsum), [b_ex], [b_ex])
                tt("dve", gates[sl], ex, gsum.unsqueeze(2).broadcast_to([128, 8, 16]), ALU.mult, [b_ex], [b_g[sl]])

            LOOK = 5

            def gather_k(sl, k):
                gt, b_gt = g_ring.next()
                P.dma("pool", lambda e: e.indirect_dma_start(
                    out=gt, out_offset=None, in_=uv16[:, :],
                    in_offset=bass.IndirectOffsetOnAxis(ap=e_i[sl][:, k:k + 1], axis=0)), [b_e[sl]] + b_conv, [b_gt])
                return gt, b_gt

            def expert_k(sl, k, gt, b_gt):
                kc = slice(k, k + 1)
                stt(junkp, gt[:, 0:D], 1.0, hf_t, ALU.mult, ALU.mult, [b_gt, b_hft], [b_jp, b_ar[sl][k]], accum=araw[sl][:, kc])
                act(gel[sl][:, kc], araw[sl][:, kc], AF.Gelu, [b_ar[sl][k]], [b_gl[sl][k]])
                dg, b_dg = diag_ring.next()
                g2 = gates[sl].rearrange("p h k -> p (h k)")
                act(gel[sl][:, kc], gel[sl][:, kc], AF.Copy, [b_gl[sl][k], b_g[sl]], [b_gl[sl][k]], scale=g2[:, kc])
                act(dg, ident_b, AF.Copy, [b_cb16, b_gl[sl][k]], [b_dg], scale=gel[sl][:, kc])
                for nb in range(4):
                    mm(acc_banks[nb][0][:, :], dg, gt[:, D + nb * 512:D + (nb + 1) * 512], k == 0, k == 127,
                       [b_dg, b_gt], [acc_banks[nb][1]])

            for _ in idx_phase(0):
                pass
            dma("sp", hf_t, hf_scr[0:128, :], [b_hfs], [b_hft])
            for i in range(NT):
                sl = i % 2
                rs = slice(i * 128, (i + 1) * 128)
                dma("sp", x3t, x3_scr[rs, :], [b_x3[i]], [b_x3t])
                nxt = idx_phase(i + 1) if i + 1 < NT else iter(())
                pend = [gather_k(sl, k) for k in range(LOOK)]
                for k in range(128):
                    if k + LOOK < 128:
                        pend.append(gather_k(sl, k + LOOK))
                    gt, b_gt = pend.pop(0)
                    expert_k(sl, k, gt, b_gt)
                    if k % 12 == 11:
                        next(nxt, None)
                for _ in nxt:
                    pass
                if i + 1 < NT:
                    dma("sp", hf_t, hf_scr[(i + 1) * 128:(i + 2) * 128, :], [b_hfs], [b_hft])
                if debug:
                    for (nm, src_) in (("dbg_e", e_i[sl]), ("dbg_g", gates[sl].rearrange("p h k -> p (h k)")), ("dbg_a", araw[sl])):
                        b_o = Buf(nm)
                        dma("sp", dbg[nm][rs, :], src_, [b_e[sl], b_g[sl]] + b_ar[sl], [b_o])
                        outbufs.append(b_o)
                for nb in range(4):
                    tt("dve", x3t[:, nb * 512:(nb + 1) * 512], acc_banks[nb][0][:, :], x3t[:, nb * 512:(nb + 1) * 512], ALU.add,
                       [acc_banks[nb][1], b_x3t], [b_x3t])
                act(junkp, x3t, AF.Square, [b_x3t], [b_jp, b_fst], accum_out=fst[:, 0:1])
                act(fst[:, 1:2], fst[:, 0:1], AF.Sqrt, [b_fst], [b_fst], scale=1.0 / D, bias=EPS)
                P.op("dve", lambda e: e.reciprocal(out=fst[:, 2:3], in_=fst[:, 1:2]), [b_fst], [b_fst])
                ot = prod.rearrange("p a b -> p (a b)")
                stt(ot, x3t, fst[:, 2:3], gfb, ALU.mult, ALU.mult, [b_x3t, b_fst, b_gfb], [b_prod])
                b_o = Buf("out%d" % i)
                dma("sp", out[rs, :], ot, [b_prod], [b_o])
                outbufs.append(b_o)

        if debug and stage == 1:
            b_o = Buf("dbg")
            dma("sp", mrg_scr[:, :, :], hT.rearrange("p a b -> a p b"), b_hT, [b_o])
            outbufs.append(b_o)

        P.wait_all("sp", outbufs)
        block = st.enter_context(nc.Block())
        P.emit(block)
    return nc


CO_IDENT = 0
CO_MASK01 = 128
CO_KS = 256
CO_QS = 264
CO_FREQ = 272
CO_B16 = 280
CO_IOTA = CO_B16 + 6 * 128
CST_W = CO_IOTA + 16


def make_consts():
    c = np.zeros((128, CST_W), np.float32)
    c[:, CO_IDENT:CO_IDENT + 128] = np.eye(128)
    j = np.arange(128)
    c[:, CO_MASK01:CO_MASK01 + 128] = (j[:, None] <= j[None, :]).astype(np.float32)
    h = np.arange(8, dtype=np.float64)
    log_g = np.log1p(-np.exp2(-5.0 - h))
    c[:, CO_KS:CO_KS + 8] = np.exp(-log_g[None, :] * (j[:, None] + 1.0)) * 128.0 ** -0.5
    c[:, CO_QS:CO_QS + 8] = np.exp(log_g[None, :] * (j[:, None] + 1.0))
    ang_r = 1.0 / (10000.0 ** np.linspace(0.0, 1.0, 64, dtype=np.float32))
    c[:, CO_FREQ] = np.repeat(ang_r, 2)
    inv = 1.0 / (10000.0 ** (np.arange(64, dtype=np.float32) / 64))
    c[:, CO_FREQ + 1] = np.concatenate([inv, inv])
    o = CO_B16
    c[:, o:o + 128] = np.eye(128)
    c[:, o + 128:o + 256] = 1.0
    pr = np.zeros((128, 128), np.float32)
    for i in range(64):
        pr[2 * i + 1, 2 * i] = -1.0
        pr[2 * i, 2 * i + 1] = 1.0
    c[:, o + 256:o + 384] = pr
    pd = np.zeros((128, 128), np.float32)
    for i in range(64):
        pd[i + 64, i] = -1.0
        pd[i, i + 64] = 1.0
    c[:, o + 384:o + 512] = pd
    c[:, o + 512:o + 640] = np.where(j[:, None] >= j[None, :], 0.0, NEG)
    c[:, o + 640:o + 768] = np.where(j[:, None] <= j[None, :], 0.0, NEG)
    c[:, CO_IOTA:CO_IOTA + 16] = np.arange(16, dtype=np.float32)[None, :]
    return c


GAMMA_C = [float(np.exp(np.log1p(-np.exp2(-5.0 - h)) * 128.0)) for h in range(8)]


def make_in_maps(inputs, ncores=8):
    cst = make_consts()
    sk = np.asarray(inputs["peer_subkeys"][0], np.float32)
    skT = np.ascontiguousarray(sk.reshape(16, 128, 128).transpose(2, 0, 1).reshape(128, 16 * 128))
    shared = {
        "cst": cst, "skT": skT,
        "g_mix": np.ascontiguousarray(inputs["g_mix"][0][None, :]),
        "w_in": np.ascontiguousarray(inputs["w_in"][0]),
        "w_br_ret": np.ascontiguousarray(inputs["w_br_ret"][0]),
        "w_br_dil": np.ascontiguousarray(inputs["w_br_dil"][0]),
        "w_out": np.ascontiguousarray(inputs["w_out"][0]),
        "g_cross": np.ascontiguousarray(inputs["g_cross"][0][None, :]),
        "g_mem": np.ascontiguousarray(inputs["g_mem"][0][None, :]),
        "w_q_mem": np.ascontiguousarray(inputs["w_q_mem"][0]),
        "w_kv_mem": np.ascontiguousarray(inputs["w_kv_mem"][0]),
        "w_o_mem": np.ascontiguousarray(inputs["w_o_mem"][0]),
        "g_ffn": np.ascontiguousarray(inputs["g_ffn"][0][None, :]),
        "w_peer_q": np.ascontiguousarray(inputs["w_peer_q"][0]),
        "peer_u": np.ascontiguousarray(inputs["peer_u"][0]),
        "peer_v": np.ascontiguousarray(inputs["peer_v"][0]),
        "g_final": np.ascontiguousarray(np.asarray(inputs["g_final"])[None, :]),
    }
    maps = []
    for b in range(ncores):
        m = dict(shared)
        m["x"] = np.ascontiguousarray(inputs["x"][b])
        m["mem"] = np.ascontiguousarray(inputs["mem"][b])
        m["pos"] = np.ascontiguousarray(np.asarray(inputs["positions"][b], np.int32)[None, :])
        maps.append(m)
    return maps


def kernel(**inputs):
    nc = build()
    maps = make_in_maps(inputs)
    res = run_bass_kernel_spmd(nc, maps, core_ids=list(range(8)))
    return np.stack([r["out"] for r in res.results], axis=0)
```

```python
import math
import numpy as np
from contextlib import ExitStack
import concourse.bass as bass
import concourse.mybir as mybir
from concourse.bass_utils import run_bass_kernel_spmd

F32 = mybir.dt.float32
BF16 = mybir.dt.bfloat16
I32 = mybir.dt.int32
U32 = mybir.dt.uint32
AF = mybir.ActivationFunctionType
ALU = mybir.AluOpType
AX = mybir.AxisListType

S = 2048
D = 2048
NT = 16
NCH = 16
EPS = 1e-6
WB = 256
NEG = -30000.0
TWO_PI = 2.0 * math.pi
CW1 = 6.28125
CW2 = TWO_PI - CW1

C_RQ, C_RK, C_RV, C_RG = 0, 1024, 2048, 3072
C_DQ, C_DK, C_DV = 4096, 5632, 7168
C_GR, C_GD = 8704, 10752

SAME_ENG_SYNC = True


class Buf:
    __slots__ = ("name", "w", "r")

    def __init__(self, name=""):
        self.name = name
        self.w = None
        self.r = {}


class Prog:
    COMPUTE = ("pe", "dve", "act", "pool")

    def __init__(self, nc, st, n_dma_sems=48):
        self.nc = nc
        self.q = {e: [] for e in ("pe", "dve", "act", "pool", "sp")}
        self.sem = {}
        for e in self.COMPUTE:
            self.sem[e] = st.enter_context(nc.semaphore("c_" + e))
        self.cnt = {e: 0 for e in self.COMPUTE}
        self.ndma = n_dma_sems
        for i in range(n_dma_sems):
            self.sem[("d", i)] = st.enter_context(nc.semaphore("d%d" % i))
        self.dma_uses = [0] * n_dma_sems
        self.dma_next = 0
        self.seen = {e: {} for e in self.q}
        self.n_ops = 0

    def _need(self, eng, dep, waits):
        if dep is None:
            return
        key, val, deng = dep
        if key == eng and not (SAME_ENG_SYNC and eng in ("dve", "act", "pool")):
            return
        if key == "pe" and eng == "pe":
            return
        if self.seen[eng].get(key, 0) >= val:
            return
        self.seen[eng][key] = val
        waits.append((key, val))

    def _deps(self, eng, reads, writes):
        waits = []
        for b in reads:
            self._need(eng, b.w, waits)
        for b in writes:
            self._need(eng, b.w, waits)
            for k, (v, e) in b.r.items():
                if k == eng and eng in self.COMPUTE:
                    continue
                self._need(eng, (k, v, e), waits)
        return waits

    def _commit(self, dep, reads, writes):
        for b in reads:
            cur = b.r.get(dep[0])
            if cur is None or cur[0] < dep[1]:
                b.r[dep[0]] = (dep[1], dep[2])
        for b in writes:
            b.w = dep
            b.r = {}

    def op(self, eng, fn, reads=(), writes=()):
        waits = self._deps(eng, reads, writes)
        self.cnt[eng] += 1
        dep = (eng, self.cnt[eng], eng)
        self.q[eng].append((waits, fn, (eng, 1)))
        self._commit(dep, reads, writes)
        self.n_ops += 1
        return dep

    def dma(self, qeng, fn, reads=(), writes=()):
        waits = self._deps(qeng, reads, writes)
        i = self.dma_next
        self.dma_next = (self.dma_next + 1) % self.ndma
        key = ("d", i)
        prev = 16 * self.dma_uses[i]
        if prev > 0:
            self._need(qeng, (key, prev, qeng), waits)
        self.dma_uses[i] += 1
        dep = (key, prev + 16, qeng)
        self.q[qeng].append((waits, fn, (key, 16)))
        self._commit(dep, reads, writes)
        self.n_ops += 1
        return dep

    def barrier(self):
        for eng in self.q:
            waits = []
            for e in self.COMPUTE:
                if self.cnt[e] > 0:
                    self._need(eng, (e, self.cnt[e], e), waits) if e != eng else None
            for i in range(self.ndma):
                if self.dma_uses[i] > 0:
                    self._need(eng, (("d", i), 16 * self.dma_uses[i], "x"), waits)
            self.q[eng].append((waits, None, None))

    def wait_all(self, eng, bufs):
        waits = []
        for b in bufs:
            self._need(eng, b.w, waits)
        self.q[eng].append((waits, None, None))

    def emit(self, block):
        handles = {"pe": block.tensor, "dve": block.vector, "act": block.scalar,
                   "pool": block.gpsimd, "sp": block.sync}
        sem = self.sem
        for e, dec in handles.items():
            items = self.q[e]

            def body(engh, items=items):
                for waits, fn, inc in items:
                    for k, v in waits:
                        engh.wait_ge(sem[k], v)
                    if fn is not None:
                        ins = fn(engh)
                        ins.then_inc(sem[inc[0]], inc[1])
            dec(body)


class Ring:
    def __init__(self, tiles, name="ring"):
        self.tiles = tiles
        self.bufs = [Buf("%s%d" % (name, i)) for i in range(len(tiles))]
        self.i = 0

    def next(self):
        t, b = self.tiles[self.i], self.bufs[self.i]
        self.i = (self.i + 1) % len(self.tiles)
        return t, b


def run_interleaved(gens):
    gens = list(gens)
    while gens:
        for g in list(gens):
            try:
                next(g)
            except StopIteration:
                gens.remove(g)


class Arena:
    def __init__(self, t, nbytes):
        self.t, self.n, self.off = t, nbytes, 0

    def alloc(self, shape, dty):
        esz = 2 if dty == BF16 else 4
        n = int(np.prod(shape[1:])) * esz
        n_al = (n + 63) // 64 * 64
        assert self.off + n_al <= self.n, ("arena overflow", self.off, n_al, self.n)
        v = self.t[:, self.off // 2:(self.off + n) // 2]
        if dty != BF16:
            v = v.bitcast(dty)
        self.off += n_al
        if len(shape) == 3:
            v = v.rearrange("p (a b) -> p a b", a=shape[1])
        return v

    def reset(self):
        self.off = 0


def build(stage=99, debug=False):
    nc = bass.Bass("TRN2", target_bir_lowering=False)

    def dt(name, shape, dty=F32, kind="ExternalInput"):
        return nc.dram_tensor(name, list(shape), dty, kind=kind).ap()

    skind = "ExternalOutput" if debug else "Internal"
    x = dt("x", [S, D])
    mem = dt("mem", [256, D])
    pos = dt("pos", [1, S], I32)
    g_mix = dt("g_mix", [1, D])
    w_in = dt("w_in", [D, 12800])
    w_br_ret = dt("w_br_ret", [1024, D])
    w_br_dil = dt("w_br_dil", [512, D])
    w_out = dt("w_out", [D, D])
    g_cross = dt("g_cross", [1, D])
    g_mem = dt("g_mem", [1, D])
    w_q_mem = dt("w_q_mem", [D, D])
    w_kv_mem = dt("w_kv_mem", [D, 2 * D])
    w_o_mem = dt("w_o_mem", [D, D])
    g_ffn = dt("g_ffn", [1, D])
    w_peer_q = dt("w_peer_q", [D, D])
    skT = dt("skT", [128, 16 * 128])
    peer_u = dt("peer_u", [16384, D])
    peer_v = dt("peer_v", [16384, D])
    g_final = dt("g_final", [1, D])
    cst = dt("cst", [128, CST_W])
    out = dt("out", [S, D], kind="ExternalOutput")

    ret_scr = dt("ret_scr", [8, 128, S], BF16, kind=skind)
    dil_scr = dt("dil_scr", [4, 128, S], BF16, kind=skind)
    mrg_scr = dt("mrg_scr", [16, 128, S], BF16, kind=skind)
    x2_scr = dt("x2_scr", [S, D], F32, kind=skind)
    x3_scr = dt("x3_scr", [S, D], F32, kind=skind)
    hf_scr = dt("hf_scr", [S, D], BF16, kind=skind)
    uv16 = dt("uv16", [16384, 2 * D], BF16, kind="Internal")
    dbg = {}
    if debug:
        dbg["dbg_e"] = dt("dbg_e", [S, 128], I32, kind="ExternalOutput")
        dbg["dbg_g"] = dt("dbg_g", [S, 128], F32, kind="ExternalOutput")
        dbg["dbg_a"] = dt("dbg_a", [S, 128], F32, kind="ExternalOutput")

    with ExitStack() as st:
        P = Prog(nc, st)

        def sb(name, shape, dty=F32):
            return st.enter_context(nc.sbuf_tensor(name, list(shape), dty))

        def ps(name, shape, dty=F32):
            return st.enter_context(nc.psum_tensor(name, list(shape), dty))

        def mm(o, lhsT, rhs, start, stop, reads, writes):
            P.op("pe", lambda e: e.matmul(o, lhsT, rhs, start=start, stop=stop), reads, writes)

        def tr(o, in_, ident, reads, writes):
            P.op("pe", lambda e: e.transpose(out=o, in_=in_, identity=ident), reads, writes)

        def act(o, in_, func, reads, writes, **kw):
            P.op("act", lambda e: e.activation(out=o, in_=in_, func=func, **kw), reads, writes)

        def tt(eng, o, a, b, op, reads, writes):
            P.op(eng, lambda e: e.tensor_tensor(out=o, in0=a, in1=b, op=op), reads, writes)

        def ts(eng, o, a, s1, s2, op0, op1, reads, writes):
            if op1 is None:
                P.op(eng, lambda e: e.tensor_scalar(out=o, in0=a, scalar1=s1, scalar2=None, op0=op0), reads, writes)
            else:
                P.op(eng, lambda e: e.tensor_scalar(out=o, in0=a, scalar1=s1, scalar2=s2, op0=op0, op1=op1), reads, writes)

        def stt(o, a, sc, b, op0, op1, reads, writes, accum=None):
            if accum is None:
                P.op("dve", lambda e: e.scalar_tensor_tensor(out=o, in0=a, scalar=sc, in1=b, op0=op0, op1=op1), reads, writes)
            else:
                P.op("dve", lambda e: e.scalar_tensor_tensor(out=o, in0=a, scalar=sc, in1=b, op0=op0, op1=op1, accum_out=accum), reads, writes)

        def cp(eng, o, in_, reads, writes):
            if eng == "act":
                act(o, in_, AF.Copy, reads, writes)
            else:
                P.op(eng, lambda e: e.tensor_copy(out=o, in_=in_), reads, writes)

        def dma(q, o, in_, reads, writes):
            P.dma(q, lambda e: e.dma_start(out=o, in_=in_), reads, writes)

        A1t = sb("A1", [128, 32768], BF16)
        A2t = sb("A2", [128, 32768], BF16)
        ACt = sb("AC", [128, 18432], BF16)
        A2 = Arena(A2t, 65536)
        AC = Arena(ACt, 36864)
        R1 = A1t[:, :].rearrange("p (a b) -> p a b", a=NCH)
        wring = Ring([sb("w%d" % i, [128, NCH, WB], BF16) for i in range(4)], "w")
        cst_sb = sb("cst_sb", [128, CST_W], F32)
        b_cst = Buf("cst")
        dma("sp", cst_sb[:], cst[:, :], [], [b_cst])
        ident_f = cst_sb[:, CO_IDENT:CO_IDENT + 128]
        mask01 = cst_sb[:, CO_MASK01:CO_MASK01 + 128]
        ks_c = cst_sb[:, CO_KS:CO_KS + 8]
        qs_c = cst_sb[:, CO_QS:CO_QS + 8]
        freq_c = cst_sb[:, CO_FREQ:CO_FREQ + 2]
        cb16 = sb("cb16", [128, 6 * 128], BF16)
        b_cb16 = Buf("cb16")
        cp("dve", cb16[:], cst_sb[:, CO_B16:CO_B16 + 6 * 128], [b_cst], [b_cb16])
        ident_b = cb16[:, 0:128]
        ones_b = cb16[:, 128:256]
        perm_b = [cb16[:, 256:384], cb16[:, 384:512]]
        dmask_b = [cb16[:, 512:640], cb16[:, 640:768]]
        stat = sb("stat", [128, 64], F32)

        pjring = Ring([ps("pj%d" % i, [128, 512], F32) for i in range(2)], "pj")
        tpring = Ring([ps("tp%d" % i, [128, 1024], BF16) for i in range(2)], "tp")
        mring = Ring([ps("m%d" % i, [128, 512], F32) for i in range(4)], "m")

        CONV_ROWS = 1024
        conv_todo = [(c0_, src_, r0) for r0 in range(0, 16384, CONV_ROWS) for (c0_, src_) in ((0, peer_u), (D, peer_v))]
        b_conv = []
        conv_state = {"n": 0}

        def conv_step():
            if stage >= 7 and conv_todo:
                c0_, src_, r0 = conv_todo.pop(0)
                b = Buf("conv")
                dma("pool", uv16[r0:r0 + CONV_ROWS, c0_:c0_ + D], src_[r0:r0 + CONV_ROWS, :], [], [b])
                b_conv.append(b)

        def load_w(wd, r0, nch, c0, ncols=WB):
            t, b = wring.next()
            src = wd[r0:r0 + nch * 128, c0:c0 + ncols].rearrange("(ch p) c -> p ch c", p=128)
            dma("pool", t[:, 0:nch, 0:ncols], src, [], [b])
            conv_state["n"] += 1
            if conv_state["n"] % 2 == 0:
                conv_step()
            return t, b

        def norm_pass(src, ntiles, g_dram, dstT, dst_bufs, arena, tm_out=None, src_bufs=None, b_tm=None, xarena=None):
            gb = arena.alloc([128, D], F32)
            b_gb = Buf("gb")
            dma("sp", gb, g_dram[0:1, :].partition_broadcast(128), [], [b_gb])
            xr = Ring([(xarena or arena).alloc([128, D], F32) for _ in range(4 if xarena is not None else 2)], "xt")
            xnr = Ring([arena.alloc([128, D], BF16) for _ in range(2)], "xn")
            junk = arena.alloc([128, D], BF16)
            b_junk = Buf("junk")
            def chain(c):
                for i in range(c, ntiles, 2):
                    xt, b_xt = xr.next()
                    xn, b_xn = xnr.next()
                    b_st = Buf("st")
                    dma("sp", xt, src[i * 128:(i + 1) * 128, :], [src_bufs[i]] if src_bufs is not None else [], [b_xt])
                    ssq = stat[:, 2 * (i % 16):2 * (i % 16) + 1]
                    rstd = stat[:, 2 * (i % 16) + 1:2 * (i % 16) + 2]
                    yield
                    act(junk, xt, AF.Square, [b_xt], [b_junk, b_st], accum_out=ssq)
                    yield
                    act(ssq, ssq, AF.Sqrt, [b_st], [b_st], scale=1.0 / D, bias=EPS)
                    yield
                    P.op("dve", lambda e, ssq=ssq, rstd=rstd: e.reciprocal(out=rstd, in_=ssq), [b_st], [b_st])
                    yield
                    stt(xn, xt, rstd, gb, ALU.mult, ALU.mult, [b_xt, b_st, b_gb], [b_xn])
                    yield
                    if tm_out is not None:
                        dma("pool", tm_out[i * 128:(i + 1) * 128, :], xn, [b_xn], [b_tm])
                    for half in range(2):
                        tp, b_tp = tpring.next()
                        tpv = tp[:, :].rearrange("p (a b) -> p a b", a=8)
                        for c8 in range(8):
                            ch = half * 8 + c8
                            tr(tpv[:, c8, :], xn[:, ch * 128:(ch + 1) * 128], ident_b, [b_xn, b_cb16], [b_tp])
                        yield
                        cp("act" if half == 0 else "dve", dstT[:, half * 8:half * 8 + 8, i * 128:(i + 1) * 128], tpv,
                           [b_tp], [dst_bufs[i]])
                        yield

            run_interleaved([chain(0), chain(1)])

        hT = R1
        b_hT = [Buf("hT%d" % i) for i in range(NT)]
        norm_pass(x, NT, g_mix, hT, b_hT, AC, xarena=A2)
        P.barrier()
        A2.reset()
        AC.reset()

        outbufs = []
        b_retscr = [Buf("retscr%d" % i) for i in range(8)]
        b_dilscr = [Buf("dilscr%d" % i) for i in range(4)]
        b_mrgscr = [Buf("mrgscr%d" % i) for i in range(16)]

        def make_tables(tmp, fcol, cos_t, sin_t, b_tab):
            posi = tmp.alloc([128, S], I32)
            tA = tmp.alloc([128, S], F32)
            tB = tmp.alloc([128, S], F32)
            tC = tmp.alloc([128, S], F32)
            tBi = tB.bitcast(I32)
            b_p, b_t = Buf("posi"), Buf("tt")
            dma("sp", posi, pos[0:1, :].partition_broadcast(128), [], [b_p])
            cp("dve", tC, posi, [b_p], [b_t])
            ts("dve", tA, tC, fcol, None, ALU.mult, None, [b_t, b_cst], [b_t])
            ts("dve", tBi, tA, 1.0 / TWO_PI, None, ALU.mult, None, [b_t], [b_t])
            cp("dve", tC, tBi, [b_t], [b_t])
            stt(tA, tC, -CW1, tA, ALU.mult, ALU.add, [b_t], [b_t])
            stt(tA, tC, -CW2, tA, ALU.mult, ALU.add, [b_t], [b_t])
            ts("dve", tC, tA, math.pi, -TWO_PI, ALU.is_gt, ALU.mult, [b_t], [b_t])
            tt("dve", tB, tA, tC, ALU.add, [b_t], [b_t])
            act(sin_t, tB, AF.Sin, [b_t], [b_tab])
            ts("dve", tA, tA, math.pi / 2, None, ALU.add, None, [b_t], [b_t])
            ts("dve", tC, tA, math.pi, -TWO_PI, ALU.is_gt, ALU.mult, [b_t], [b_t])
            tt("dve", tB, tA, tC, ALU.add, [b_t], [b_t])
            act(cos_t, tB, AF.Sin, [b_t], [b_tab])

        def proj_rot(wt, b_w, wc0, raw_ring, cos_t, sin_t, b_tab, perm, dst, b_dst, t1r, t2r):
            raw, b_raw = raw_ring.next()
            for tb in range(4):
                pj, b_pj = pjring.next()
                for ch in range(NCH):
                    mm(pj[:, :], wt[:, ch, wc0:wc0 + 128], hT[:, ch, tb * 512:(tb + 1) * 512], ch == 0, ch == NCH - 1,
                       [b_w] + b_hT[tb * 4:tb * 4 + 4], [b_pj])
                cp("act", raw[:, tb * 512:(tb + 1) * 512], pj[:, :], [b_pj], [b_raw])
            for tb in range(4):
                sl = slice(tb * 512, (tb + 1) * 512)
                m, b_m = mring.next()
                mm(m[:, :], perm, raw[:, sl], True, True, [b_raw, b_cb16], [b_m])
                t1, b_t1 = t1r.next()
                t2, b_t2 = t2r.next()
                tt("pool", t1, raw[:, sl], cos_t[:, sl], ALU.mult, [b_raw, b_tab], [b_t1])
                tt("dve", t2, m[:, :], sin_t[:, sl], ALU.mult, [b_m, b_tab], [b_t2])
                tt("dve", dst[:, sl], t1, t2, ALU.add, [b_t1, b_t2], [b_dst])

        if stage >= 2:
            cos_r = A2.alloc([128, S], F32)
            sin_r = A2.alloc([128, S], F32)
            b_tabr = Buf("tabr")
            make_tables(AC, freq_c[:, 0:1], cos_r, sin_r, b_tabr)
            P.barrier()
            AC.reset()
            raw_ring = Ring([A2.alloc([128, S], BF16) for _ in range(2)], "raw")
            qT = [A2.alloc([128, S], BF16) for _ in range(2)]
            kT = [A2.alloc([128, S], BF16) for _ in range(2)]
            b_qT = [Buf("qT0"), Buf("qT1")]
            b_kT = [Buf("kT0"), Buf("kT1")]
            v_sb = A2.alloc([128, NT, 256], BF16)
            rgs = A2.alloc([128, NT, 256], BF16)
            b_v = [Buf("v%d" % i) for i in range(NT)]
            b_rg = [Buf("rg%d" % i) for i in range(NT)]
            t1r = Ring([AC.alloc([128, 512], F32) for _ in range(2)], "t1")
            t2r = Ring([AC.alloc([128, 512], F32) for _ in range(2)], "t2")
            retT_ring = Ring([AC.alloc([128, S], BF16) for _ in range(2)], "retT")
            U = [AC.alloc([128, 128], F32) for _ in range(2)]
            b_U = [Buf("U0"), Buf("U1")]
            Sb_ring = [Ring([AC.alloc([128, 128], BF16) for _ in range(2)], "Sb%d" % i) for i in range(2)]
            kp_ring = Ring([AC.alloc([128, 128], BF16) for _ in range(3)], "kp")
            sc_ring = Ring([AC.alloc([128, 128], BF16) for _ in range(3)], "sc")
            rt_ring = Ring([AC.alloc([128, 128], BF16) for _ in range(3)], "rt")
            junk_s = AC.alloc([128, 128], BF16)
            b_junk_s = Buf("junk_s")
            st_ring = Ring([stat[:, 32 + 4 * i:32 + 4 * i + 4] for i in range(8)], "rst")

            for pr in range(4):
                h0 = 2 * pr
                wq, b_wq = load_w(w_in, 0, NCH, C_RQ + h0 * 128)
                wk, b_wk = load_w(w_in, 0, NCH, C_RK + h0 * 128)
                wv, b_wv = load_w(w_in, 0, NCH, C_RV + h0 * 128)
                wg, b_wg = load_w(w_in, 0, NCH, C_RG + h0 * 128)
                for hh in range(2):
                    proj_rot(wq, b_wq, hh * 128, raw_ring, cos_r, sin_r, b_tabr, perm_b[0], qT[hh], b_qT[hh], t1r, t2r)
                    proj_rot(wk, b_wk, hh * 128, raw_ring, cos_r, sin_r, b_tabr, perm_b[0], kT[hh], b_kT[hh], t1r, t2r)
                v_done = [0]

                def vtile(i, wv=wv, b_wv=b_wv, wg=wg, b_wg=b_wg, v_done=v_done):
                    pj, b_pj = pjring.next()
                    for ch in range(NCH):
                        mm(pj[:, 0:256], hT[:, ch, i * 128:(i + 1) * 128], wv[:, ch, :], ch == 0, ch == NCH - 1,
                           [b_wv, b_hT[i]], [b_pj])
                        if ch % 4 == 3:
                            yield
                    cp("act", v_sb[:, i, :], pj[:, 0:256], [b_pj], [b_v[i]])
                    pj, b_pj = pjring.next()
                    for ch in range(NCH):
                        mm(pj[:, 0:256], hT[:, ch, i * 128:(i + 1) * 128], wg[:, ch, :], ch == 0, ch == NCH - 1,
                           [b_wg, b_hT[i]], [b_pj])
                        if ch % 4 == 3:
                            yield
                    act(rgs[:, i, :], pj[:, 0:256], AF.Silu, [b_pj], [b_rg[i]])
                    v_done[0] = i + 1
                    yield

                def vchain():
                    for i in range(1, NT):
                        yield from vtile(i)

                for _ in vtile(0):
                    pass
                retT = []
                for hh in range(2):
                    retT.append(retT_ring.next())
                def ret_chain(hh):
                    h = h0 + hh
                    Sb_prev = None
                    for n in range(NT):
                        while v_done[0] <= n:
                            yield
                        csl = slice(n * 128, (n + 1) * 128)
                        vsl = v_sb[:, n, hh * 128:(hh + 1) * 128]
                        tp, b_tp = tpring.next()
                        tr(tp[:, 0:128], kT[hh][:, csl], ident_b, [b_kT[hh], b_cb16], [b_tp])
                        m, b_m = mring.next()
                        mm(m[:, 0:128], kT[hh][:, csl], qT[hh][:, csl], True, True, [b_kT[hh], b_qT[hh]], [b_m])
                        yield
                        kp, b_kp = kp_ring.next()
                        act(kp, tp[:, 0:128], AF.Copy, [b_tp, b_cst], [b_kp], scale=ks_c[:, h:h + 1])
                        sc, b_sc = sc_ring.next()
                        stt(sc, m[:, 0:128], ks_c[:, h:h + 1], mask01, ALU.mult, ALU.mult, [b_m, b_cst], [b_sc])
                        yield
                        mo, b_mo = mring.next()
                        mm(mo[:, 0:128], sc, vsl, True, n == 0, [b_sc, b_v[n]], [b_mo])
                        if n > 0:
                            mm(mo[:, 0:128], qT[hh][:, csl], Sb_prev[0], False, True, [b_qT[hh], Sb_prev[1]], [b_mo])
                        mk, b_mk = mring.next()
                        mm(mk[:, 0:128], kp, vsl, True, True, [b_kp, b_v[n]], [b_mk])
                        yield
                        if n == 0:
                            cp("dve", U[hh], mk[:, 0:128], [b_mk], [b_U[hh]])
                        else:
                            stt(U[hh], U[hh], GAMMA_C[h], mk[:, 0:128], ALU.mult, ALU.add, [b_mk, b_U[hh]], [b_U[hh]])
                        stq, b_stq = st_ring.next()
                        act(junk_s, mo[:, 0:128], AF.Square, [b_mo, b_cst], [b_junk_s, b_stq], scale=qs_c[:, h:h + 1],
                            accum_out=stq[:, 0:1])
                        yield
                        if n < NT - 1:
                            Sb, b_Sb = Sb_ring[hh].next()
                            act(Sb, U[hh], AF.Copy, [b_U[hh]], [b_Sb], scale=GAMMA_C[h])
                            Sb_prev = (Sb, b_Sb)
                        act(stq[:, 1:2], stq[:, 0:1], AF.Sqrt, [b_stq], [b_stq], scale=1.0 / 128, bias=EPS)
                        yield
                        P.op("dve", lambda e, stq=stq: e.reciprocal(out=stq[:, 2:3], in_=stq[:, 1:2]), [b_stq], [b_stq])
                        yield
                        tt("dve", stq[:, 3:4], stq[:, 2:3], qs_c[:, h:h + 1], ALU.mult, [b_stq, b_cst], [b_stq])
                        yield
                        rt, b_rt = rt_ring.next()
                        stt(rt, mo[:, 0:128], stq[:, 3:4], rgs[:, n, hh * 128:(hh + 1) * 128], ALU.mult, ALU.mult,
                            [b_mo, b_stq, b_rg[n]], [b_rt])
                        yield
                        tp, b_tp = tpring.next()
                        tr(tp[:, 0:128], rt, ident_b, [b_rt, b_cb16], [b_tp])
                        yield
                        cp("act", retT[hh][0][:, csl], tp[:, 0:128], [b_tp], [retT[hh][1]])
                        yield

                run_interleaved([ret_chain(0), ret_chain(1), vchain()])
                for hh in range(2):
                    dma("sp", ret_scr[h0 + hh, :, :], retT[hh][0], [retT[hh][1]], [b_retscr[h0 + hh]])
                    outbufs.append(b_retscr[h0 + hh])
            P.barrier()
            A2.reset()
            AC.reset()

        if stage >= 3:
            cos_d = A2.alloc([128, S], F32)
            sin_d = A2.alloc([128, S], F32)
            b_tabd = Buf("tabd")
            make_tables(AC, freq_c[:, 1:2], cos_d, sin_d, b_tabd)
            P.barrier()
            AC.reset()
            raw_ring = Ring([A2.alloc([128, S], BF16) for _ in range(2)], "raw")
            qT = [A2.alloc([128, S], BF16) for _ in range(2)]
            kT = [A2.alloc([128, S], BF16) for _ in range(2)]
            b_qT = [Buf("dqT0"), Buf("dqT1")]
            b_kT = [Buf("dkT0"), Buf("dkT1")]
            vblk = A2.alloc([128, 16, 256], BF16)
            b_vb = [Buf("vb%d" % i) for i in range(16)]
            t1r = Ring([A2.alloc([128, 512], F32) for _ in range(2)], "t1")
            t2r = Ring([A2.alloc([128, 512], F32) for _ in range(2)], "t2")
            pT_ring = Ring([A2.alloc([128, 256], BF16) for _ in range(3)], "pT")
            acc_n = [AC.alloc([128, S], F32) for _ in range(2)]
            acc_d = [AC.alloc([128, S], F32) for _ in range(2)]
            b_an = [Buf("an0"), Buf("an1")]
            b_ad = [Buf("ad0"), Buf("ad1")]
            DSCALE = 128.0 ** -0.5
            scring = Ring([t[:, :].bitcast(F32) for t in tpring.tiles], "scr")
            scring.bufs = tpring.bufs

            def blk_cols(g, bi):
                if g == 0:
                    return slice(bi * 128, (bi + 1) * 128)
                if g == 1:
                    c, n = bi // 4, bi % 4
                    s0 = c + n * 128 * 4
                    return slice(s0, s0 + 127 * 4 + 1, 4)
                return slice(bi, bi + 127 * 16 + 1, 16)

            def has_prev(g, bi):
                return (g == 0 and bi > 0) or (g == 1 and bi % 4 > 0)

            def acc_view(acc, g, k):
                if g == 0:
                    return acc[:, k * 512:(k + 1) * 512]
                if g == 1:
                    return acc[:, k:k + 4 * 511 + 1:4]
                return acc.rearrange("p (i c) -> p c i", c=16)[:, 4 * k:4 * k + 4, :]

            for jp in range(2):
                for g in range(3):
                    hd0 = g * 4 + 2 * jp
                    wq, b_wq = load_w(w_in, 0, NCH, C_DQ + hd0 * 128)
                    wk, b_wk = load_w(w_in, 0, NCH, C_DK + hd0 * 128)
                    wv, b_wv = load_w(w_in, 0, NCH, C_DV + hd0 * 128)
                    for hh in range(2):
                        proj_rot(wq, b_wq, hh * 128, raw_ring, cos_d, sin_d, b_tabd, perm_b[1], qT[hh], b_qT[hh], t1r, t2r)
                        proj_rot(wk, b_wk, hh * 128, raw_ring, cos_d, sin_d, b_tabd, perm_b[1], kT[hh], b_kT[hh], t1r, t2r)
                    vb_done = [0]

                    def vblock(bi, g=g, wv=wv, b_wv=b_wv, vb_done=vb_done):
                        cols = blk_cols(g, bi)
                        pj, b_pj = pjring.next()
                        for ch in range(NCH):
                            mm(pj[:, 0:256], hT[:, ch, cols], wv[:, ch, :], ch == 0, ch == NCH - 1, [b_wv] + b_hT, [b_pj])
                            if ch % 4 == 3:
                                yield
                        cp("act", vblk[:, bi, :], pj[:, 0:256], [b_pj], [b_vb[bi]])
                        vb_done[0] = bi + 1
                        yield

                    def vbchain():
                        for bi in range(4, 16):
                            yield from vblock(bi)

                    def att_chain(g=g, vb_done=vb_done):
                        for k in range(4):
                            for hh in range(2):
                                mn, b_mn = mring.next()
                                md, b_md = mring.next()
                                for blk in range(4):
                                    bi = 4 * k + blk
                                    while vb_done[0] <= bi:
                                        yield
                                    qc = blk_cols(g, bi)
                                    hp = has_prev(g, bi)
                                    sc, b_sc = scring.next()
                                    if hp:
                                        pc = blk_cols(g, bi - 1)
                                        mm(sc[:, 0:128], kT[hh][:, pc], qT[hh][:, qc], True, False, [b_kT[hh], b_qT[hh]], [b_sc])
                                        mm(sc[:, 0:128], ident_b, dmask_b[0], False, True, [b_cb16], [b_sc])
                                    mm(sc[:, 128:256], kT[hh][:, qc], qT[hh][:, qc], True, False, [b_kT[hh], b_qT[hh]], [b_sc])
                                    mm(sc[:, 128:256], ident_b, dmask_b[1], False, True, [b_cb16], [b_sc])
                                    yield
                                    pT, b_pT = pT_ring.next()
                                    lo = 0 if hp else 128
                                    act(pT[:, lo:256], sc[:, lo:256], AF.Exp, [b_sc], [b_pT], scale=DSCALE)
                                    yield
                                    osl = slice(blk * 128, (blk + 1) * 128)
                                    vs = slice(hh * 128, (hh + 1) * 128)
                                    if hp:
                                        mm(mn[:, osl], vblk[:, bi - 1, vs], pT[:, 0:128], True, False, [b_vb[bi - 1], b_pT], [b_mn])
                                        mm(md[:, osl], ones_b, pT[:, 0:128], True, False, [b_cb16, b_pT], [b_md])
                                    mm(mn[:, osl], vblk[:, bi, vs], pT[:, 128:256], not hp, True, [b_vb[bi], b_pT], [b_mn])
                                    mm(md[:, osl], ones_b, pT[:, 128:256], not hp, True, [b_cb16, b_pT], [b_md])
                                    yield
                                for (mp, b_mp, acc, b_acc) in ((mn, b_mn, acc_n[hh], b_an[hh]), (md, b_md, acc_d[hh], b_ad[hh])):
                                    av = acc_view(acc, g, k)
                                    mv = mp[:, :] if g < 2 else mp[:, :].rearrange("p (c i) -> p c i", c=4)
                                    if g == 0:
                                        cp("act", av, mv, [b_mp], [b_acc])
                                    else:
                                        tt("dve", av, mv, av, ALU.add, [b_mp, b_acc], [b_acc])
                                    yield

                    for bi in range(4):
                        for _ in vblock(bi):
                            pass
                    run_interleaved([att_chain(), vbchain()])
                for hh in range(2):
                    P.op("dve", lambda e, hh=hh: e.reciprocal(out=acc_d[hh], in_=acc_d[hh]), [b_ad[hh]], [b_ad[hh]])
                    stg, b_stg = raw_ring.next()
                    tt("dve", stg, acc_n[hh], acc_d[hh], ALU.mult, [b_an[hh], b_ad[hh]], [b_stg])
                    dma("sp", dil_scr[2 * jp + hh, :, :], stg, [b_stg], [b_dilscr[2 * jp + hh]])
                    outbufs.append(b_dilscr[2 * jp + hh])
            P.barrier()
            A2.reset()
            AC.reset()

        R2 = A2t[:, :].rearrange("p (a b) -> p a b", a=NCH)
        b_R1 = [Buf("R1_%d" % i) for i in range(NT)]
        b_R2 = [Buf("R2_%d" % i) for i in range(NT)]
        b_x2 = [Buf("x2_%d" % i) for i in range(NT)]
        b_x3 = [Buf("x3_%d" % i) for i in range(NT)]
        b_hfs = Buf("hf_scr")

        if stage >= 4:
            retT_all = A2.alloc([128, 8, S], BF16)
            dilT_all = A2.alloc([128, 4, S], BF16)
            b_rta, b_dta = Buf("rta"), Buf("dta")
            for h in range(8):
                dma("sp", retT_all[:, h, :], ret_scr[h, :, :], [b_retscr[h]], [b_rta])
            for j in range(4):
                dma("sp", dilT_all[:, j, :], dil_scr[j, :, :], [b_dilscr[j]], [b_dta])
            sg_ring = Ring([AC.alloc([128, 512], F32) for _ in range(4)], "sg")
            ta_ring = Ring([AC.alloc([128, 512], F32) for _ in range(4)], "ta")
            mst_ring = Ring([AC.alloc([128, S], BF16) for _ in range(2)], "mst")
            wbr_ring = Ring([AC.alloc([128, 8, WB], BF16) for _ in range(2)], "wbr")
            wbd_ring = Ring([AC.alloc([128, 4, WB], BF16) for _ in range(2)], "wbd")
            for mc2 in range(8):
                wgr, b_wgr = load_w(w_in, 0, NCH, C_GR + mc2 * 256)
                wgd, b_wgd = load_w(w_in, 0, NCH, C_GD + mc2 * 256)
                wbr, b_wbr = wbr_ring.next()
                dma("pool", wbr, w_br_ret[:, mc2 * 256:(mc2 + 1) * 256].rearrange("(ch p) c -> p ch c", p=128), [], [b_wbr])
                wbd, b_wbd = wbd_ring.next()
                dma("pool", wbd, w_br_dil[:, mc2 * 256:(mc2 + 1) * 256].rearrange("(ch p) c -> p ch c", p=128), [], [b_wbd])
                for sub in range(2):
                    mc = 2 * mc2 + sub
                    wsl = slice(sub * 128, (sub + 1) * 128)
                    mst, b_mst = mst_ring.next()
                    for tb in range(4):
                        tsl = slice(tb * 512, (tb + 1) * 512)
                        hb = b_hT[tb * 4:tb * 4 + 4]
                        sgs = []
                        for (wg_, b_wg_) in ((wgr, b_wgr), (wgd, b_wgd)):
                            pj, b_pj = pjring.next()
                            for ch in range(NCH):
                                mm(pj[:, :], wg_[:, ch, wsl], hT[:, ch, tsl], ch == 0, ch == NCH - 1, [b_wg_] + hb, [b_pj])
                            sg, b_sg = sg_ring.next()
                            act(sg, pj[:, :], AF.Sigmoid, [b_pj], [b_sg])
                            sgs.append((sg, b_sg))
                        m1, b_m1 = mring.next()
                        for kc in range(8):
                            mm(m1[:, :], wbr[:, kc, wsl], retT_all[:, kc, tsl], kc == 0, kc == 7, [b_wbr, b_rta], [b_m1])
                        m2, b_m2 = mring.next()
                        for kc in range(4):
                            mm(m2[:, :], wbd[:, kc, wsl], dilT_all[:, kc, tsl], kc == 0, kc == 3, [b_wbd, b_dta], [b_m2])
                        ta, b_ta = ta_ring.next()
                        tb_, b_tb = ta_ring.next()
                        tt("dve", ta, m1[:, :], sgs[0][0], ALU.mult, [b_m1, sgs[0][1]], [b_ta])
                        tt("dve", tb_, m2[:, :], sgs[1][0], ALU.mult, [b_m2, sgs[1][1]], [b_tb])
                        tt("dve", mst[:, tsl], ta, tb_, ALU.add, [b_ta, b_tb], [b_mst])
                    dma("sp", mrg_scr[mc, :, :], mst, [b_mst], [b_mrgscr[mc]])
                    outbufs.append(b_mrgscr[mc])
            P.barrier()
            A2.reset()
            AC.reset()

        def resid_proj(actT, b_act, wd, resid, b_res, dst, b_dst):
            RB = 512
            w2ring = Ring([A2.alloc([128, NCH, RB], BF16) for _ in range(4)], "w2")
            xp_ring = Ring([AC.alloc([128, RB], F32) for _ in range(4)], "xp")
            op_ring = Ring([AC.alloc([128, RB], F32) for _ in range(4)], "op")
            for cb in range(D // RB):
                w, b_w = w2ring.next()
                dma("pool", w, wd[:, cb * RB:(cb + 1) * RB].rearrange("(ch p) c -> p ch c", p=128), [], [b_w])
                for i in range(NT):
                    rs = slice(i * 128, (i + 1) * 128)
                    pj, b_pj = pjring.next()
                    for kc in range(NCH):
                        mm(pj[:, :], actT[:, kc, rs], w[:, kc, :], kc == 0, kc == NCH - 1, [b_w, b_act[i]], [b_pj])
                    xp, b_xp = xp_ring.next()
                    dma("sp", xp, resid[rs, cb * RB:(cb + 1) * RB], [b_res[i]] if b_res is not None else [], [b_xp])
                    o, b_o = op_ring.next()
                    tt("dve", o, pj[:, :], xp, ALU.add, [b_pj, b_xp], [b_o])
                    dma("act", dst[rs, cb * RB:(cb + 1) * RB], o, [b_o], [b_dst[i]])

        if stage >= 5:
            for mc in range(16):
                dma("sp", R1[:, mc, :], mrg_scr[mc, :, :], [b_mrgscr[mc]] + b_hT, b_R1)
            resid_proj(R1, b_R1, w_out, x, None, x2_scr, b_x2)
            outbufs.extend(b_x2)
            P.barrier()
            AC.reset()
            A2.reset()
            norm_pass(x2_scr, NT, g_cross, R2, b_R2, AC, src_bufs=b_x2, xarena=Arena(A1t, 65536))
            P.barrier()
            AC.reset()

        if stage >= 6:
            A1 = Arena(A1t, 65536)
            memnT = A1.alloc([128, NCH, 256], BF16)
            b_mn = [Buf("mn0"), Buf("mn1")]
            norm_pass(mem, 2, g_mem, memnT, b_mn, AC)
            P.barrier()
            AC.reset()
            kmT = AC.alloc([128, NCH, 256], BF16)
            vm = AC.alloc([128, 2, D], BF16)
            b_km, b_vm = Buf("km"), Buf("vm")
            kvring = Ring([A1.alloc([128, NCH, 512], BF16) for _ in range(3)], "kvw")
            for cb in range(D // 512):
                w, b_w = kvring.next()
                dma("pool", w, w_kv_mem[:, cb * 512:(cb + 1) * 512].rearrange("(ch p) c -> p ch c", p=128), [], [b_w])
                for sub in range(4):
                    pj, b_pj = pjring.next()
                    for ch in range(NCH):
                        mm(pj[:, 0:256], w[:, ch, sub * 128:(sub + 1) * 128], memnT[:, ch, :], ch == 0, ch == NCH - 1,
                           [b_w] + b_mn, [b_pj])
                    cp("act", kmT[:, 4 * cb + sub, :], pj[:, 0:256], [b_pj], [b_km])
            for cb in range(D // 512):
                w, b_w = kvring.next()
                dma("pool", w, w_kv_mem[:, D + cb * 512:D + (cb + 1) * 512].rearrange("(ch p) c -> p ch c", p=128), [], [b_w])
                for mt in range(2):
                    pj, b_pj = pjring.next()
                    for ch in range(NCH):
                        mm(pj[:, :], memnT[:, ch, mt * 128:(mt + 1) * 128], w[:, ch, :], ch == 0, ch == NCH - 1,
                           [b_w, b_mn[mt]], [b_pj])
                    cp("act", vm[:, mt, cb * 512:(cb + 1) * 512], pj[:, :], [b_pj], [b_vm])
            P.barrier()
            oT = R1
            b_oT = [Buf("oT%d" % i) for i in range(NT)]
            qm = AC.alloc([128, 4, S], BF16)
            b_qm = Buf("qm")
            p_ring = Ring([AC.alloc([128, 256], F32) for _ in range(2)], "p")
            pn_ring = Ring([AC.alloc([128, 256], BF16) for _ in range(2)], "pn")
            pt_ring = Ring([AC.alloc([128, 256], BF16) for _ in range(2)], "pt")
            cst_ring = Ring([stat[:, 32 + 4 * i:32 + 4 * i + 4] for i in range(8)], "cst")
            MSCALE = 512.0 ** -0.5
            for hm in range(4):
                for wb in range(2):
                    w, b_w = load_w(w_q_mem, 0, NCH, hm * 512 + wb * WB)
                    for sub in range(2):
                        for tb in range(4):
                            pj, b_pj = pjring.next()
                            for ch in range(NCH):
                                mm(pj[:, :], w[:, ch, sub * 128:(sub + 1) * 128], R2[:, ch, tb * 512:(tb + 1) * 512],
                                   ch == 0, ch == NCH - 1, [b_w] + b_R2[tb * 4:tb * 4 + 4], [b_pj])
                            cp("act", qm[:, 2 * wb + sub, tb * 512:(tb + 1) * 512], pj[:, :], [b_pj], [b_qm])
                def att_chain(c0, hm=hm):
                    for i in range(c0, NT, 2):
                        rs = slice(i * 128, (i + 1) * 128)
                        m, b_m = mring.next()
                        for c in range(4):
                            mm(m[:, 0:256], qm[:, c, rs], kmT[:, hm * 4 + c, :], c == 0, c == 3, [b_qm, b_km], [b_m])
                        yield
                        sq, b_sq = cst_ring.next()
                        P.op("dve", lambda e, m=m, sq=sq: e.reduce_max(out=sq[:, 0:1], in_=m[:, 0:256], axis=AX.X), [b_m], [b_sq])
                        yield
                        ts("dve", sq[:, 1:2], sq[:, 0:1], -MSCALE, None, ALU.mult, None, [b_sq], [b_sq])
                        yield
                        p, b_p = p_ring.next()
                        act(p, m[:, 0:256], AF.Exp, [b_m, b_sq], [b_p, b_sq], scale=MSCALE, bias=sq[:, 1:2], accum_out=sq[:, 2:3])
                        yield
                        P.op("dve", lambda e, sq=sq: e.reciprocal(out=sq[:, 3:4], in_=sq[:, 2:3]), [b_sq], [b_sq])
                        yield
                        pn, b_pn = pn_ring.next()
                        act(pn, p, AF.Copy, [b_p, b_sq], [b_pn], scale=sq[:, 3:4])
                        yield
                        tp, b_tp = tpring.next()
                        for mt in range(2):
                            tr(tp[:, mt * 128:(mt + 1) * 128], pn[:, mt * 128:(mt + 1) * 128], ident_b, [b_pn, b_cb16], [b_tp])
                        yield
                        pt, b_pt = pt_ring.next()
                        cp("act", pt, tp[:, 0:256], [b_tp], [b_pt])
                        yield
                        mo, b_mo = mring.next()
                        for c in range(4):
                            for mt in range(2):
                                mm(mo[:, c * 128:(c + 1) * 128], vm[:, mt, (hm * 4 + c) * 128:(hm * 4 + c + 1) * 128],
                                   pt[:, mt * 128:(mt + 1) * 128], mt == 0, mt == 1, [b_vm, b_pt], [b_mo])
                        yield
                        cp("dve", oT[:, hm * 4:hm * 4 + 4, rs], mo[:, :].rearrange("p (c t) -> p c t", c=4), [b_mo], [b_oT[i]])
                        yield

                run_interleaved([att_chain(0), att_chain(1)])
            P.barrier()
            AC.reset()
            A2.reset()
            resid_proj(oT, b_oT, w_o_mem, x2_scr, b_x2, x3_scr, b_x3)
            outbufs.extend(b_x3)
            P.barrier()
            AC.reset()
            A2.reset()
            norm_pass(x3_scr, NT, g_ffn, R2, b_R2, AC, src_bufs=b_x3, tm_out=hf_scr, b_tm=b_hfs, xarena=Arena(A1t, 65536))
            outbufs.append(b_hfs)
            P.barrier()
            AC.reset()

        if stage >= 7:
            qpT = R1
            b_qp = Buf("qpT")
            for cb in range(D // WB):
                w, b_w = load_w(w_peer_q, 0, NCH, cb * WB)
                for sub in range(2):
                    for tb in range(4):
                        pj, b_pj = pjring.next()
                        for ch in range(NCH):
                            mm(pj[:, :], w[:, ch, sub * 128:(sub + 1) * 128], R2[:, ch, tb * 512:(tb + 1) * 512],
                               ch == 0, ch == NCH - 1, [b_w] + b_R2[tb * 4:tb * 4 + 4], [b_pj])
                        cp("act", qpT[:, 2 * cb + sub, tb * 512:(tb + 1) * 512], pj[:, :], [b_pj], [b_qp])
            while conv_todo:
                conv_step()
            P.barrier()
            A2.reset()
            AC.reset()
            iota16 = cst_sb[:, CO_IOTA:CO_IOTA + 16]
            sk_sb = A2.alloc([128, 16, 128], BF16)
            b_sk = Buf("sk")
            dma("pool", sk_sb, skT[:, :].rearrange("p (a n) -> p a n", a=16), [], [b_sk])
            gfb = A2.alloc([128, D], F32)
            b_gfb = Buf("gfb")
            dma("sp", gfb, g_final[0:1, :].partition_broadcast(128), [], [b_gfb])
            sc_sb = A2.alloc([128, 16, 128], F32)
            combo = A2.alloc([128, 8, 256], F32)
            onehot = A2.alloc([128, 128, 16], F32)
            prod = A2.alloc([128, 128, 16], F32)
            b_sc, b_combo, b_oh, b_prod = Buf("sc"), Buf("combo"), Buf("oh"), Buf("prod")
            vtop = A2.alloc([128, 16, 16], F32)
            itop = A2.alloc([128, 16, 16], U32)
            itopf = A2.alloc([128, 16, 16], F32)
            b_vt, b_it, b_itf = Buf("vt"), Buf("it"), Buf("itf")
            work_ring = Ring([A2.alloc([128, 128], F32) for _ in range(1)], "work")
            cwork_ring = Ring([A2.alloc([128, 256], F32) for _ in range(1)], "cwork")
            cval = A2.alloc([128, 8, 16], F32)
            cidx = A2.alloc([128, 8, 16], U32)
            b_cv, b_ci = Buf("cv"), Buf("ci")
            ab_i = [A2.alloc([128, 128], U32) for _ in range(2)]
            ab_f = [A2.alloc([128, 128], F32) for _ in range(2)]
            i12f = [A2.alloc([128, 128], F32) for _ in range(2)]
            b_ab = Buf("ab")
            e_f = A2.alloc([128, 128], F32)
            ex = A2.alloc([128, 8, 16], F32)
            gsum = A2.alloc([128, 8], F32)
            b_ef, b_ex = Buf("ef"), Buf("ex")
            e_i = [A2.alloc([128, 128], I32) for _ in range(2)]
            gates = [A2.alloc([128, 8, 16], F32) for _ in range(2)]
            araw = [A2.alloc([128, 128], F32) for _ in range(2)]
            gel = [A2.alloc([128, 128], F32) for _ in range(2)]
            b_e = [Buf("e0"), Buf("e1")]
            b_g = [Buf("g0"), Buf("g1")]
            b_ar = [[Buf() for _ in range(128)] for _ in range(2)]
            b_gl = [[Buf() for _ in range(128)] for _ in range(2)]
            hf_t = AC.alloc([128, D], BF16)
            b_hft = Buf("hft")
            junkp = A2.alloc([128, D], BF16)
            b_jp = Buf("junkp")
            diag_ring = Ring([A2.alloc([128, 128], BF16) for _ in range(4)], "diag")
            x3t = AC.alloc([128, D], F32)
            b_x3t = Buf("x3t")
            wmem = [t[:, :, :].rearrange("p a b -> p (a b)") for t in wring.tiles]
            g_ring = Ring([AC.alloc([128, 2 * D], BF16) for _ in range(3)] + wmem, "gth")
            fst = stat[:, 32:36]
            b_fst = Buf("fst")
            acc_banks = [mring.next() for _ in range(4)]

            def top16(src, vout, iout, wring_, b_src, b_v, b_i):
                wk, b_wk = wring_.next()
                P.op("dve", lambda e: e.max(out=vout[:, 0:8], in_=src), [b_src], [b_v])
                P.op("dve", lambda e: e.match_replace(out=wk, in_to_replace=vout[:, 0:8], in_values=src, imm_value=-1e30),
                     [b_src, b_v], [b_wk])
                P.op("dve", lambda e: e.max(out=vout[:, 8:16], in_=wk), [b_wk], [b_v])
                P.op("dve", lambda e: e.max_index(out=iout[:, 0:8], in_max=vout[:, 0:8], in_values=src), [b_src, b_v], [b_i])
                P.op("dve", lambda e: e.max_index(out=iout[:, 8:16], in_max=vout[:, 8:16], in_values=src), [b_src, b_v], [b_i])

            def idx_phase(i):
                sl = i % 2
                rs = slice(i * 128, (i + 1) * 128)
                for grp in range(4):
                    pj, b_pj = pjring.next()
                    for q4 in range(4):
                        hp = grp * 4 + q4
                        mm(pj[:, q4 * 128:(q4 + 1) * 128], qpT[:, hp, rs], sk_sb[:, hp, :], True, True, [b_qp, b_sk], [b_pj])
                    cp("act", sc_sb[:, grp * 4:grp * 4 + 4, :], pj[:, :].rearrange("p (a n) -> p a n", a=4), [b_pj], [b_sc])
                yield
                for hp in range(16):
                    top16(sc_sb[:, hp, :], vtop[:, hp, :], itop[:, hp, :], work_ring, b_sc, b_vt, b_it)
                    if hp % 4 == 3:
                        yield
                cp("dve", itopf, itop, [b_it], [b_itf])
                vt4 = vtop.rearrange("p (h q) k -> p h q k", q=2)
                itf4 = itopf.rearrange("p (h q) k -> p h q k", q=2)
                combo4 = combo.rearrange("p h (a b) -> p h a b", a=16)
                tt("dve", combo4, vt4[:, :, 0, :].unsqueeze(3).broadcast_to([128, 8, 16, 16]),
                   vt4[:, :, 1, :].unsqueeze(2).broadcast_to([128, 8, 16, 16]), ALU.add, [b_vt], [b_combo])
                for h in range(8):
                    top16(combo[:, h, :], cval[:, h, :], cidx[:, h, :], cwork_ring, b_combo, b_cv, b_ci)
                    if h % 4 == 3:
                        yield
                cidx2 = cidx.rearrange("p h k -> p (h k)")
                ts("dve", ab_i[0], cidx2, 4, None, ALU.logical_shift_right, None, [b_ci], [b_ab])
                ts("dve", ab_i[1], cidx2, 15, None, ALU.bitwise_and, None, [b_ci], [b_ab])
                for q in range(2):
                    cp("dve", ab_f[q], ab_i[q], [b_ab], [b_ab])
                    tt("dve", onehot, ab_f[q].unsqueeze(2).broadcast_to([128, 128, 16]),
                       iota16.unsqueeze(1).broadcast_to([128, 128, 16]), ALU.is_equal, [b_ab, b_cst], [b_oh])
                    tt("dve", prod.rearrange("p (h k) j -> p h k j", h=8), onehot.rearrange("p (h k) j -> p h k j", h=8),
                       itf4[:, :, q, :].unsqueeze(2).broadcast_to([128, 8, 16, 16]), ALU.mult, [b_oh, b_itf], [b_prod])
                    P.op("dve", lambda e, q=q: e.tensor_reduce(out=i12f[q], in_=prod, axis=AX.X, op=ALU.add), [b_prod], [b_ab])
                    yield
                stt(e_f, i12f[0], 128.0, i12f[1], ALU.mult, ALU.add, [b_ab], [b_ef])
                cp("dve", e_i[sl], e_f, [b_ef], [b_e[sl]])
                tt("dve", ex, cval, cval[:, :, 0:1].broadcast_to([128, 8, 16]), ALU.subtract, [b_cv], [b_ex])
                act(ex, ex, AF.Exp, [b_ex], [b_ex])
                P.op("dve", lambda e: e.tensor_reduce(out=gsum, in_=ex, axis=AX.X, op=ALU.add), [b_ex], [b_ex])
                P.op("dve", lambda e: e.reciprocal(out=gsum, in_=gsum), [b_ex], [b_ex])
                tt("dve", gates[sl], ex, gsum.unsqueeze(2).broadcast_to([128, 8, 16]), ALU.mult, [b_ex], [b_g[sl]])

            LOOK = 5

            def gather_k(sl, k):
                gt, b_gt = g_ring.next()
                P.dma("pool", lambda e: e.indirect_dma_start(
                    out=gt, out_offset=None, in_=uv16[:, :],
                    in_offset=bass.IndirectOffsetOnAxis(ap=e_i[sl][:, k:k + 1], axis=0)), [b_e[sl]] + b_conv, [b_gt])
                return gt, b_gt

            def expert_k(sl, k, gt, b_gt):
                kc = slice(k, k + 1)
                stt(junkp, gt[:, 0:D], 1.0, hf_t, ALU.mult, ALU.mult, [b_gt, b_hft], [b_jp, b_ar[sl][k]], accum=araw[sl][:, kc])
                act(gel[sl][:, kc], araw[sl][:, kc], AF.Gelu, [b_ar[sl][k]], [b_gl[sl][k]])
                dg, b_dg = diag_ring.next()
                g2 = gates[sl].rearrange("p h k -> p (h k)")
                act(gel[sl][:, kc], gel[sl][:, kc], AF.Copy, [b_gl[sl][k], b_g[sl]], [b_gl[sl][k]], scale=g2[:, kc])
                act(dg, ident_b, AF.Copy, [b_cb16, b_gl[sl][k]], [b_dg], scale=gel[sl][:, kc])
                for nb in range(4):
                    mm(acc_banks[nb][0][:, :], dg, gt[:, D + nb * 512:D + (nb + 1) * 512], k == 0, k == 127,
                       [b_dg, b_gt], [acc_banks[nb][1]])

            for _ in idx_phase(0):
                pass
            dma("sp", hf_t, hf_scr[0:128, :], [b_hfs], [b_hft])
            for i in range(NT):
                sl = i % 2
                rs = slice(i * 128, (i + 1) * 128)
                dma("sp", x3t, x3_scr[rs, :], [b_x3[i]], [b_x3t])
                nxt = idx_phase(i + 1) if i + 1 < NT else iter(())
                pend = [gather_k(sl, k) for k in range(LOOK)]
                for k in range(128):
                    if k + LOOK < 128:
                        pend.append(gather_k(sl, k + LOOK))
                    gt, b_gt = pend.pop(0)
                    expert_k(sl, k, gt, b_gt)
                    if k % 12 == 11:
                        next(nxt, None)
                for _ in nxt:
                    pass
                if i + 1 < NT:
                    dma("sp", hf_t, hf_scr[(i + 1) * 128:(i + 2) * 128, :], [b_hfs], [b_hft])
                if debug:
                    for (nm, src_) in (("dbg_e", e_i[sl]), ("dbg_g", gates[sl].rearrange("p h k -> p (h k)")), ("dbg_a", araw[sl])):
                        b_o = Buf(nm)
                        dma("sp", dbg[nm][rs, :], src_, [b_e[sl], b_g[sl]] + b_ar[sl], [b_o])
                        outbufs.append(b_o)
                for nb in range(4):
                    tt("dve", x3t[:, nb * 512:(nb + 1) * 512], acc_banks[nb][0][:, :], x3t[:, nb * 512:(nb + 1) * 512], ALU.add,
                       [acc_banks[nb][1], b_x3t], [b_x3t])
                act(junkp, x3t, AF.Square, [b_x3t], [b_jp, b_fst], accum_out=fst[:, 0:1])
                act(fst[:, 1:2], fst[:, 0:1], AF.Sqrt, [b_fst], [b_fst], scale=1.0 / D, bias=EPS)
                P.op("dve", lambda e: e.reciprocal(out=fst[:, 2:3], in_=fst[:, 1:2]), [b_fst], [b_fst])
                ot = prod.rearrange("p a b -> p (a b)")
                stt(ot, x3t, fst[:, 2:3], gfb, ALU.mult, ALU.mult, [b_x3t, b_fst, b_gfb], [b_prod])
                b_o = Buf("out%d" % i)
                dma("sp", out[rs, :], ot, [b_prod], [b_o])
                outbufs.append(b_o)

        if debug and stage == 1:
            b_o = Buf("dbg")
            dma("sp", mrg_scr[:, :, :], hT.rearrange("p a b -> a p b"), b_hT, [b_o])
            outbufs.append(b_o)

        P.wait_all("sp", outbufs)
        block = st.enter_context(nc.Block())
        P.emit(block)
    return nc


CO_IDENT = 0
CO_MASK01 = 128
CO_KS = 256
CO_QS = 264
CO_FREQ = 272
CO_B16 = 280
CO_IOTA = CO_B16 + 6 * 128
CST_W = CO_IOTA + 16


def make_consts():
    c = np.zeros((128, CST_W), np.float32)
    c[:, CO_IDENT:CO_IDENT + 128] = np.eye(128)
    j = np.arange(128)
    c[:, CO_MASK01:CO_MASK01 + 128] = (j[:, None] <= j[None, :]).astype(np.float32)
    h = np.arange(8, dtype=np.float64)
    log_g = np.log1p(-np.exp2(-5.0 - h))
    c[:, CO_KS:CO_KS + 8] = np.exp(-log_g[None, :] * (j[:, None] + 1.0)) * 128.0 ** -0.5
    c[:, CO_QS:CO_QS + 8] = np.exp(log_g[None, :] * (j[:, None] + 1.0))
    ang_r = 1.0 / (10000.0 ** np.linspace(0.0, 1.0, 64, dtype=np.float32))
    c[:, CO_FREQ] = np.repeat(ang_r, 2)
    inv = 1.0 / (10000.0 ** (np.arange(64, dtype=np.float32) / 64))
    c[:, CO_FREQ + 1] = np.concatenate([inv, inv])
    o = CO_B16
    c[:, o:o + 128] = np.eye(128)
    c[:, o + 128:o + 256] = 1.0
    pr = np.zeros((128, 128), np.float32)
    for i in range(64):
        pr[2 * i + 1, 2 * i] = -1.0
        pr[2 * i, 2 * i + 1] = 1.0
    c[:, o + 256:o + 384] = pr
    pd = np.zeros((128, 128), np.float32)
    for i in range(64):
        pd[i + 64, i] = -1.0
        pd[i, i + 64] = 1.0
    c[:, o + 384:o + 512] = pd
    c[:, o + 512:o + 640] = np.where(j[:, None] >= j[None, :], 0.0, NEG)
    c[:, o + 640:o + 768] = np.where(j[:, None] <= j[None, :], 0.0, NEG)
    c[:, CO_IOTA:CO_IOTA + 16] = np.arange(16, dtype=np.float32)[None, :]
    return c


GAMMA_C = [float(np.exp(np.log1p(-np.exp2(-5.0 - h)) * 128.0)) for h in range(8)]


def make_in_maps(inputs, ncores=8):
    cst = make_consts()
    sk = np.asarray(inputs["peer_subkeys"][0], np.float32)
    skT = np.ascontiguousarray(sk.reshape(16, 128, 128).transpose(2, 0, 1).reshape(128, 16 * 128))
    shared = {
        "cst": cst, "skT": skT,
        "g_mix": np.ascontiguousarray(inputs["g_mix"][0][None, :]),
        "w_in": np.ascontiguousarray(inputs["w_in"][0]),
        "w_br_ret": np.ascontiguousarray(inputs["w_br_ret"][0]),
        "w_br_dil": np.ascontiguousarray(inputs["w_br_dil"][0]),
        "w_out": np.ascontiguousarray(inputs["w_out"][0]),
        "g_cross": np.ascontiguousarray(inputs["g_cross"][0][None, :]),
        "g_mem": np.ascontiguousarray(inputs["g_mem"][0][None, :]),
        "w_q_mem": np.ascontiguousarray(inputs["w_q_mem"][0]),
        "w_kv_mem": np.ascontiguousarray(inputs["w_kv_mem"][0]),
        "w_o_mem": np.ascontiguousarray(inputs["w_o_mem"][0]),
        "g_ffn": np.ascontiguousarray(inputs["g_ffn"][0][None, :]),
        "w_peer_q": np.ascontiguousarray(inputs["w_peer_q"][0]),
        "peer_u": np.ascontiguousarray(inputs["peer_u"][0]),
        "peer_v": np.ascontiguousarray(inputs["peer_v"][0]),
        "g_final": np.ascontiguousarray(np.asarray(inputs["g_final"])[None, :]),
    }
    maps = []
    for b in range(ncores):
        m = dict(shared)
        m["x"] = np.ascontiguousarray(inputs["x"][b])
        m["mem"] = np.ascontiguousarray(inputs["mem"][b])
        m["pos"] = np.ascontiguousarray(np.asarray(inputs["positions"][b], np.int32)[None, :])
        maps.append(m)
    return maps


def kernel(**inputs):
    nc = build()
    maps = make_in_maps(inputs)
    res = run_bass_kernel_spmd(nc, maps, core_ids=list(range(8)))
    return np.stack([r["out"] for r in res.results], axis=0)
```
